# Optimizing a Trainium2 kernel written in Bass

```python
import math
import jax, jax.numpy as jnp
from jax import lax
import numpy as np

D_MODEL = 1024
BATCH = 8
SEQ = 4096
DEPTH = 1

RET_HEADS = 4
RET_QK_DIM = 128
RET_V_DIM = 256
RET_CHUNK = 128
RET_QK_WIDTH = RET_HEADS * RET_QK_DIM
RET_V_WIDTH = RET_HEADS * RET_V_DIM
ROPE_BASE = 10000.0
SGU_GROUPS = 4
SGU_GROUP_DIM = 256
SGU_CHUNK = 128
SGU_WIDTH = SGU_GROUPS * SGU_GROUP_DIM
IN_WIDTHS = (RET_QK_WIDTH, RET_QK_WIDTH, RET_V_WIDTH, RET_V_WIDTH, SGU_WIDTH, SGU_WIDTH, D_MODEL, D_MODEL)
IN_WIDTH = 7168
IN_SPLITS = (512, 1024, 2048, 3072, 4096, 5120, 6144)
PEER_HEADS = 8
PEER_N_KEYS = 128
PEER_N_EXPERTS = PEER_N_KEYS * PEER_N_KEYS
PEER_KEY_DIM = 256
PEER_HALF = PEER_KEY_DIM // 2
PEER_TOPK = 16
PEER_TOKEN_BLOCK = 128
N_MOD = 6
EPS = 1e-6

kernel_name = "hybrid_retention_sgu_peer_block"


def rms_norm(x, gain):
    xf = x.astype(jnp.float32)
    y = xf * lax.rsqrt(jnp.mean(xf * xf, axis=-1, keepdims=True) + EPS)
    return (y * gain.astype(jnp.float32)).astype(x.dtype)


def modulate(h, shift, scale):
    return h * (1 + scale[:, None, :]) + shift[:, None, :]


def rotary(x, pos):
    d = x.shape[-1]
    half = d // 2
    inv = ROPE_BASE ** (-jnp.arange(half, dtype=jnp.float32) * 2.0 / d)
    ang = pos[:, None] * inv[None, :]
    cos = jnp.cos(ang)[None, :, None, :].astype(x.dtype)
    sin = jnp.sin(ang)[None, :, None, :].astype(x.dtype)
    x1, x2 = x[..., :half], x[..., half:]
    return jnp.concatenate([x1 * cos - x2 * sin, x1 * sin + x2 * cos], axis=-1)


def retention(q, k, v):
    B, S, H, dk = q.shape
    dv = v.shape[-1]
    C = RET_CHUNK
    nc = S // C
    gamma = 1.0 - 2.0 ** (-5.0 - jnp.arange(H, dtype=jnp.float32))
    log_g = jnp.log(gamma)
    idx = jnp.arange(C, dtype=jnp.float32)
    diff = idx[:, None] - idx[None, :]
    decay_in = jnp.where((diff >= 0)[None], jnp.exp(log_g[:, None, None] * jnp.maximum(diff, 0.0)[None]), 0.0).astype(q.dtype)
    decay_q = jnp.exp(log_g[None, :] * (idx[:, None] + 1.0)).astype(q.dtype)[None, :, :, None]
    decay_k = jnp.exp(log_g[None, :] * (C - 1.0 - idx[:, None])).astype(q.dtype)[None, :, :, None]
    decay_chunk = jnp.exp(log_g * C).astype(q.dtype)[None, :, None, None]

    def to_chunks(t):
        return t.reshape(B, nc, C, H, t.shape[-1]).transpose(1, 0, 2, 3, 4)

    def step(state, inp):
        qi, ki, vi = inp
        s = jnp.einsum('bihd,bjhd->bhij', qi, ki) * decay_in
        inner = jnp.einsum('bhij,bjhv->bihv', s, vi)
        cross = jnp.einsum('bihd,bhdv->bihv', qi, state) * decay_q
        new_state = state * decay_chunk + jnp.einsum('bjhd,bjhv->bhdv', ki * decay_k, vi)
        return new_state, inner + cross

    state0 = jnp.zeros((B, H, dk, dv), q.dtype)
    _, y = lax.scan(step, state0, (to_chunks(q), to_chunks(k), to_chunks(v)))
    return y.transpose(1, 0, 2, 3, 4).reshape(B, S, H, dv)


def head_norm(y, gain):
    B, S = y.shape[:2]
    yf = y.astype(jnp.float32)
    mu = jnp.mean(yf, axis=-1, keepdims=True)
    var = jnp.mean(jnp.square(yf - mu), axis=-1, keepdims=True)
    yn = ((yf - mu) * lax.rsqrt(var + EPS)).reshape(B, S, -1)
    return (yn * gain.astype(jnp.float32)).astype(y.dtype)


def spatial_gating(u, sv, ln_g, ln_b, w_s, b_s):
    B, S, _ = u.shape
    vf = sv.astype(jnp.float32)
    mu = jnp.mean(vf, axis=-1, keepdims=True)
    var = jnp.mean(jnp.square(vf - mu), axis=-1, keepdims=True)
    vn = ((vf - mu) * lax.rsqrt(var + EPS) * ln_g.astype(jnp.float32) + ln_b.astype(jnp.float32)).astype(sv.dtype)
    nck = S // SGU_CHUNK
    vn = vn.reshape(B, nck, SGU_CHUNK, SGU_GROUPS, SGU_GROUP_DIM)
    mask = jnp.tril(jnp.ones((SGU_CHUNK, SGU_CHUNK), dtype=bool))
    ws = jnp.where(mask[None], w_s, jnp.zeros_like(w_s))
    mixed = jnp.einsum('gts,bnsgc->bntgc', ws, vn) + b_s.T[None, None, :, :, None]
    return u * mixed.reshape(B, S, SGU_WIDTH)


def peer(h, w_q, sub_keys, expert_u, expert_v):
    B, S, D = h.shape
    blocks = h.reshape(-1, PEER_TOKEN_BLOCK, D)
    K = PEER_TOPK

    def block(hb):
        T = hb.shape[0]
        q = (hb @ w_q).reshape(T, PEER_HEADS, 2, PEER_HALF)
        s = jnp.einsum('thpc,hpnc->thpn', q, sub_keys)
        sub_s, sub_i = lax.top_k(s, K)
        cand = (sub_s[:, :, 0, :, None] + sub_s[:, :, 1, None, :]).reshape(T, PEER_HEADS, K * K)
        top_s, top_i = lax.top_k(cand, K)
        k1 = jnp.take_along_axis(sub_i[:, :, 0], top_i // K, axis=-1)
        k2 = jnp.take_along_axis(sub_i[:, :, 1], top_i % K, axis=-1)
        expert = k1 * PEER_N_KEYS + k2
        g = jax.nn.softmax(top_s.astype(jnp.float32), axis=-1).astype(hb.dtype)
        ue = expert_u[expert]
        act = jax.nn.gelu(jnp.einsum('td,thkd->thk', hb, ue), approximate=False) * g
        ve = expert_v[expert]
        return jnp.einsum('thk,thkd->td', act, ve)

    return lax.map(block, blocks).reshape(B, S, D)


def setup_inputs(seed: int = 0) -> dict:
    key = jax.random.key(seed)
    ks = jax.random.split(key, 21)
    L, D = DEPTH, D_MODEL
    f32 = jnp.float32
    nrm = lambda k, shape, s: jax.random.normal(k, shape, f32) * s
    return {
        "x": nrm(ks[0], (BATCH, SEQ, D), 1.0),
        "c": nrm(ks[1], (BATCH, D), 1.0),
        "w_ada": nrm(ks[2], (L, D, N_MOD * D), 0.5 * D ** -0.5),
        "b_ada": nrm(ks[3], (L, N_MOD * D), 0.01),
        "norm1_g": 1.0 + nrm(ks[4], (L, D), 0.02),
        "w_in": nrm(ks[5], (L, D, IN_WIDTH), D ** -0.5),
        "ret_gn_g": 1.0 + nrm(ks[6], (L, RET_V_WIDTH), 0.02),
        "sgu_ln_g": 1.0 + nrm(ks[7], (L, SGU_WIDTH), 0.02),
        "sgu_ln_b": nrm(ks[8], (L, SGU_WIDTH), 0.01),
        "sgu_w": nrm(ks[9], (L, SGU_GROUPS, SGU_CHUNK, SGU_CHUNK), 0.5 * SGU_CHUNK ** -0.5),
        "sgu_b": 1.0 + nrm(ks[10], (L, SGU_GROUPS, SGU_CHUNK), 0.01),
        "w_ret_out": nrm(ks[11], (L, RET_V_WIDTH, D), RET_V_WIDTH ** -0.5),
        "w_sgu_out": nrm(ks[12], (L, SGU_WIDTH, D), SGU_WIDTH ** -0.5),
        "w_out": nrm(ks[13], (L, D, D), D ** -0.5),
        "norm2_g": 1.0 + nrm(ks[14], (L, D), 0.02),
        "peer_w_q": nrm(ks[15], (L, D, PEER_HEADS * PEER_KEY_DIM), D ** -0.5),
        "peer_sub_keys": nrm(ks[16], (L, PEER_HEADS, 2, PEER_N_KEYS, PEER_HALF), PEER_HALF ** -0.5),
        "peer_u": nrm(ks[17], (L, PEER_N_EXPERTS, D), D ** -0.5),
        "peer_v": nrm(ks[18], (L, PEER_N_EXPERTS, D), 1.0),
        "final_g": 1.0 + nrm(ks[19], (D,), 0.02),
    }


def reference(x, c, w_ada, b_ada, norm1_g, w_in, ret_gn_g, sgu_ln_g, sgu_ln_b, sgu_w, sgu_b, w_ret_out, w_sgu_out, w_out, norm2_g, peer_w_q, peer_sub_keys, peer_u, peer_v, final_g):
    B, S, D = x.shape
    pos = jnp.arange(S, dtype=jnp.float32)
    c_act = jax.nn.silu(c)
    for l in range(DEPTH):
        mod = c_act @ w_ada[l] + b_ada[l]
        shift1, scale1, gate1, shift2, scale2, gate2 = jnp.split(mod, N_MOD, axis=-1)

        h = modulate(rms_norm(x, norm1_g[l]), shift1, scale1)
        proj = h @ w_in[l]
        q, k, v, g_ret, u, sv, gate_a, gate_b = jnp.split(proj, IN_SPLITS, axis=-1)

        q = rotary(q.reshape(B, S, RET_HEADS, RET_QK_DIM), pos)
        k = rotary(k.reshape(B, S, RET_HEADS, RET_QK_DIM), pos) * (RET_QK_DIM ** -0.5)
        v = v.reshape(B, S, RET_HEADS, RET_V_DIM)
        y_ret = head_norm(retention(q, k, v), ret_gn_g[l])
        branch_a = (jax.nn.silu(g_ret) * y_ret) @ w_ret_out[l]

        y_sgu = spatial_gating(jax.nn.gelu(u, approximate=False), jax.nn.gelu(sv, approximate=False),
                               sgu_ln_g[l], sgu_ln_b[l], sgu_w[l], sgu_b[l])
        branch_b = y_sgu @ w_sgu_out[l]

        merged = jax.nn.sigmoid(gate_a) * branch_a + jax.nn.sigmoid(gate_b) * branch_b
        x = x + gate1[:, None, :] * (merged @ w_out[l])

        h2 = modulate(rms_norm(x, norm2_g[l]), shift2, scale2)
        x = x + gate2[:, None, :] * peer(h2, peer_w_q[l], peer_sub_keys[l], peer_u[l], peer_v[l])
    return rms_norm(x, final_g)
```

```python
import numpy as np
from contextlib import ExitStack
import ml_dtypes
import concourse.bass as bass
import concourse.mybir as mybir
from concourse.bass_utils import run_bass_kernel_spmd

F32 = mybir.dt.float32
BF16 = mybir.dt.bfloat16
I32 = mybir.dt.int32
U32 = mybir.dt.uint32
AF = mybir.ActivationFunctionType
ALU = mybir.AluOpType
AX = mybir.AxisListType

D = 1024
KT = 8
NPIECE = 14
NPA = 18
EPS = 1e-6
NSLOT = 12
ARENA = 41 * 1024
INTERLEAVE = True


class Tk:
    __slots__ = ("a", "w", "r", "al", "name", "lay", "off", "size")

    def __init__(self, a, name=""):
        self.a = a
        self.w = None
        self.r = {}
        self.al = []
        self.name = name
        self.lay = None
        self.off = 0
        self.size = 0


class Sched:
    ENG = ("pe", "act", "dve", "pool", "sp")

    def __init__(self, nc, es):
        self.nc = nc
        self.es = es
        self.q = {e: [] for e in self.ENG}
        self.sem = {e: es.enter_context(nc.semaphore("s_" + e)) for e in self.ENG}
        self.cnt = {}
        self.known = {e: {} for e in self.ENG}
        self.dsem = {}
        self.arena = None
        self.lay_off = {}
        self.carved = []
        self.nins = {e: 0 for e in self.ENG}
        self.rec = None

    def tile(self, name, shape, dt):
        t = self.es.enter_context(self.nc.sbuf_tensor(name, list(shape), dt))
        return Tk(t[:], name)

    def psum(self, name, shape, dt):
        t = self.es.enter_context(self.nc.psum_tensor(name, list(shape), dt))
        return Tk(t[:], name)

    def carve(self, lay, name, nbytes, dt, pattern=None, **kw):
        if self.arena is None:
            self.arena = self.es.enter_context(self.nc.sbuf_tensor("arena", [128, ARENA // 4], F32))
        off = self.lay_off.get(lay, 0)
        nbytes = (nbytes + 31) // 32 * 32
        assert off + nbytes <= ARENA, (lay, name, off, nbytes)
        self.lay_off[lay] = off + nbytes
        a = self.arena[:, off // 4:(off + nbytes) // 4]
        if dt != F32:
            a = a.bitcast(dt)
        if pattern is not None:
            a = a.rearrange(pattern, **kw)
        t = Tk(a, name)
        t.lay, t.off, t.size = lay, off, nbytes
        for u in self.carved:
            if u.lay != lay and u.off < off + nbytes and off < u.off + u.size:
                u.al.append(t)
                t.al.append(u)
        self.carved.append(t)
        return t

    def sub(self, parent, a, name=""):
        t = Tk(a, name or parent.name)
        t.al = list(parent.al)
        for u in parent.al:
            u.al.append(t)
        return t

    @staticmethod
    def alias(xs, ys):
        for x in xs:
            for y in ys:
                x.al.append(y)
                y.al.append(x)

    def _dsem(self, key):
        if key not in self.dsem:
            self.dsem[key] = self.es.enter_context(self.nc.semaphore("d_" + key))
        return self.dsem[key]

    def op(self, eng, fn, reads=(), writes=(), dma=None):
        if self.rec is not None:
            self.rec.append((eng, fn, list(reads), list(writes), dma))
            return None
        deps = {}

        def add(d):
            if d is not None and deps.get(d[0], 0) < d[1]:
                deps[d[0]] = d[1]

        for t in reads:
            add(t.w)
            for u in t.al:
                add(u.w)
        for t in writes:
            add(t.w)
            for s, v in t.r.items():
                add((s, v))
            for u in t.al:
                add(u.w)
                for s, v in u.r.items():
                    add((s, v))
        own = self.sem[eng]
        waits = []
        kn = self.known[eng]
        for s, v in deps.items():
            if eng == "pe" and s is own:
                continue
            if kn.get(s, 0) >= v:
                continue
            kn[s] = v
            waits.append((s, v))
        if dma is None:
            s, inc = own, 1
        else:
            s, inc = self._dsem(dma), 16
        val = self.cnt.get(s, 0) + inc
        self.cnt[s] = val
        self.nins[eng] += 1 + len(waits)

        def emit(e, waits=waits, fn=fn, s=s, inc=inc):
            for ws, wv in waits:
                e.wait_ge(ws, wv)
            fn(e).then_inc(s, inc)

        self.q[eng].append(emit)
        for t in writes:
            t.w = (s, val)
            t.r = {}
        for t in reads:
            if t.r.get(s, 0) < val:
                t.r[s] = val
        return (s, val)

    def finish(self, final_deps):
        def emit(e):
            for s, v in final_deps:
                e.wait_ge(s, v)
        self.q["sp"].append(emit)

    def run(self):
        with self.nc.Block() as block:
            @block.tensor
            def _(e):
                for f in self.q["pe"]:
                    f(e)

            @block.scalar
            def _(e):
                for f in self.q["act"]:
                    f(e)

            @block.vector
            def _(e):
                for f in self.q["dve"]:
                    f(e)

            @block.gpsimd
            def _(e):
                for f in self.q["pool"]:
                    f(e)

            @block.sync
            def _(e):
                for f in self.q["sp"]:
                    f(e)


def build_program(NCH, gC):
    nc = bass.Bass("TRN2", target_bir_lowering=False)

    def din(name, shape, dt=F32):
        return nc.dram_tensor(name, list(shape), dt, kind="ExternalInput").ap()

    T = NCH * 128
    x_d = din("x", [T, D])
    out_d = nc.dram_tensor("out", [T, D], F32, kind="ExternalOutput").ap()
    ccol_d = din("ccol", [128, 8])
    wada_d = din("w_ada", [D, 6 * D])
    bada_d = din("b_ada", [1, 6 * D])
    cols_d = din("cols", [128, 40])
    win_d = din("w_in", [D, 7168])
    wr_d = din("w_ret_out", [D, D])
    ws_d = din("w_sgu_out", [D, D])
    wo_d = din("w_out", [D, D])
    wq_d = din("w_q", [D, 2048])
    keys_d = din("keys", [16, 128, 128])
    sguw_d = din("sgu_w", [4, 128, 128])
    sgub_d = din("sgu_b", [1, 512])
    fg_d = din("final_g", [1, D])
    pu_d = din("peer_u", [16384, D])
    pv_d = din("peer_v", [16384, D])
    identb_d = din("identb", [128, 128], BF16)
    cmisc_d = din("cmisc", [128, 408])
    cs_d = din("cs", [NCH, 128, 256])
    winb_d = nc.dram_tensor("winb", [NPA, 128, KT * 512], BF16).ap()
    puv_d = nc.dram_tensor("puv", [16384, 2 * D], BF16).ap()

    with ExitStack() as es:
        S = Sched(nc, es)
        Wr = S.tile("Wr", [128, KT, D], BF16)
        Ws = S.tile("Ws", [128, KT, D], BF16)
        Wo = S.tile("Wo", [128, KT, D], BF16)
        wbuf = [S.tile("wbuf%d" % i, [128, KT, 512], BF16) for i in range(2)]
        keysT = S.tile("keysT", [128, 16, 128], BF16)
        wsT = S.tile("wsT", [128, 4, 128], BF16)
        bsBp = S.tile("bsBp", [128, 8, 128], F32)
        gate2b = S.tile("gate2b", [128, D], F32)
        fgb = S.tile("fgb", [128, D], F32)
        state = S.tile("state", [128, D], F32)
        stateb = S.tile("stateb", [128, D], BF16)
        identb = S.tile("identb_s", [128, 128], BF16)
        onesb = S.tile("onesb", [128, 128], BF16)
        cm = S.tile("cmisc_s", [128, 408], F32)
        cols = S.tile("cols_s", [128, 40], F32)
        modcol = S.tile("modcol", [128, 32], F32)
        acol = S.tile("acol", [128, 16], F32)
        xts = [S.tile("xt%d" % i, [128, D], F32) for i in range(3)]
        junkA = S.tile("junkA", [128, D], BF16)
        junkDs = [S.tile("junkD0", [128, D], BF16)] * 2
        sm = S.tile("smalls", [128, 64], F32)
        h2toks = [S.tile("h2tok%d" % i, [128, D], BF16) for i in range(2)]
        eidxs = [S.tile("eidx%d" % i, [128, 128], I32) for i in range(2)]
        ggs = [S.tile("gg%d" % i, [128, 8, 16], F32) for i in range(2)]
        pre = S.tile("pre", [128, 128], F32)
        glt = S.tile("glt", [128, 128], F32)
        glg = S.tile("glg", [128, 128], F32)
        dg = [S.tile("dg%d" % i, [128, 128], BF16) for i in range(4)]
        rtmp = S.tile("rtmp", [128, 512], F32)
        slots = [S.tile("slot%d" % i, [128, 2 * D], BF16) for i in range(NSLOT)]
        banks = [S.psum("bank%d" % i, [128, 512], F32) for i in range(8)]
        bank_rr = [0]

        def nb(n=1):
            r = []
            for _ in range(n):
                r.append(banks[bank_rr[0] % 4])
                bank_rr[0] += 1
            return r if n > 1 else r[0]

        trilT = cm.a[:, 0:128]
        dq = cm.a[:, 128:132]
        dk = cm.a[:, 132:136]
        iota16 = cm.a[:, 136:152]
        ones_f = cm.a[:, 152:280]
        identf = cm.a[:, 280:408]
        g1col, g2col = cols.a[:, 0:8], cols.a[:, 8:16]
        gncol, lngcol, lnbcol = cols.a[:, 16:24], cols.a[:, 24:32], cols.a[:, 32:40]
        sh1col, sc1col = modcol.a[:, 0:8], modcol.a[:, 8:16]
        sh2col, sc2col = modcol.a[:, 16:24], modcol.a[:, 24:32]
        a1col, a2col = acol.a[:, 0:8], acol.a[:, 8:16]

        ccol = S.carve("P1", "ccol", 32, F32)
        cact = S.carve("P1", "cact", 32, F32)
        wab = [S.carve("P1", "wab%d" % i, 4096, F32, "p (k n) -> p k n", k=KT) for i in range(2)]
        modrow = S.carve("P1", "modrow", 24576, F32)
        gate1b = S.carve("P1", "gate1b", 4096, F32)
        stage = [S.carve("P1", "stage0", 4096, F32)] * 2
        kst = S.carve("P2", "kst", 8192, F32, "p (k n) -> p k n", k=16)
        kstb = S.carve("P2", "kstb", 4096, BF16, "p (k n) -> p k n", k=16)
        wst = S.carve("P2", "wst", 2048, F32, "p (k n) -> p k n", k=4)
        wstb = S.carve("P2", "wstb", 1024, BF16, "p (k n) -> p k n", k=4)
        bsBs = S.carve("P2", "bsBs", 2048, F32, "p (k n) -> p k n", k=4)
        bsrow = S.carve("P2", "bsrow", 2048, F32)
        xn = S.carve("M", "xn", 2048, BF16)
        hT = S.carve("M", "hT", 2048, BF16, "p (k n) -> p k n", k=KT)
        cs = S.carve("M", "cs", 1024, F32)
        qk = S.carve("M", "qk", 4096, F32, "p (h n) -> p h n", h=8)
        rA = S.carve("M", "rA", 4096, F32, "p (h n) -> p h n", h=8)
        rB = S.carve("M", "rB", 4096, F32, "p (h n) -> p h n", h=8)
        qd = S.carve("M", "qd", 1024, BF16, "p (h n) -> p h n", h=4)
        kp = S.carve("M", "kp", 1024, BF16, "p (h n) -> p h n", h=4)
        qkT = S.carve("M", "qkT", 2048, BF16, "p (h n) -> p h n", h=8)
        vv = S.carve("M", "v", 2048, BF16)
        sg = S.carve("M", "sg", 4096, F32)
        guT = S.carve("M", "guT", 2048, BF16, "p (k n) -> p k n", k=KT)
        gsv = S.carve("M", "gsv", 4096, F32)
        vhat = S.carve("M", "vhat", 2048, BF16)
        sigA = S.carve("M", "sigA", 2048, BF16, "p (k n) -> p k n", k=KT)
        sigB = S.carve("M", "sigB", 2048, BF16, "p (k n) -> p k n", k=KT)
        sTm = S.carve("M", "sTm", 1024, BF16, "p (h n) -> p h n", h=4)
        zz = xn
        zT = hT
        ysT = qkT
        mergedT = guT
        xn2 = S.carve("E", "xn2", 2048, BF16)
        h2T = S.carve("E", "h2T", 2048, BF16, "p (k n) -> p k n", k=KT)
        qT = S.carve("E", "qT", 4096, BF16, "p (k n) -> p k n", k=16)
        W8 = S.carve("E", "W8", 8192, F32)
        v1 = S.carve("E", "v1", 1024, F32, "p (k n) -> p k n", k=16)
        i1 = S.carve("E", "i1", 1024, U32, "p (k n) -> p k n", k=16)
        i1f = S.carve("E", "i1f", 1024, F32, "p (k n) -> p k n", k=16)
        tsv = S.carve("E", "ts", 512, F32, "p (k n) -> p k n", k=8)
        tiv = S.carve("E", "ti", 512, U32, "p (k n) -> p k n", k=8)
        tab = S.carve("E", "tab", 1024, U32)
        abf = S.carve("E", "abf", 1024, F32)
        k12 = S.carve("E", "k12", 1024, F32)
        ef = S.carve("E", "ef", 512, F32)
        gex = S.carve("E", "gex", 512, F32, "p (k n) -> p k n", k=8)
        s3 = W8.a.rearrange("p (k n) -> p k n", k=16)
        s_hp = [S.sub(W8, s3[:, hp, :], "s%d" % hp) for hp in range(16)]
        c3 = W8.a.rearrange("p (k n) -> p k n", k=8)
        c_h = [S.sub(W8, c3[:, h, :], "c%d" % h) for h in range(8)]
        oh = S.sub(W8, W8.a.rearrange("p (h k j) -> p h k j", h=8, k=16), "oh")
        S.alias(s_hp, c_h)
        S.alias(s_hp, [oh])
        S.alias(c_h, [oh])
        v1_hp = [S.sub(v1, v1.a[:, hp, :]) for hp in range(16)]
        i1_hp = [S.sub(i1, i1.a[:, hp, :]) for hp in range(16)]
        ts_h = [S.sub(tsv, tsv.a[:, h, :]) for h in range(8)]
        ti_h = [S.sub(tiv, tiv.a[:, h, :]) for h in range(8)]
        pre_c = [S.sub(pre, pre.a[:, j:j + 1]) for j in range(128)]
        gl_c = [S.sub(glt, glt.a[:, j:j + 1]) for j in range(128)]
        glg_c = [S.sub(glg, glg.a[:, j:j + 1]) for j in range(128)]
        ss = Tk(sm.a[:, 0:1], "ss")
        rstd = Tk(sm.a[:, 1:2], "rstd")
        bst = Tk(sm.a[:, 2:26].rearrange("p (h s) -> p h s", h=4), "bst")
        bmv = Tk(sm.a[:, 26:34].rearrange("p (h s) -> p h s", h=4), "bmv")
        brs = Tk(sm.a[:, 34:38], "brs")
        lst = Tk(sm.a[:, 38:50], "lst")
        lmv = Tk(sm.a[:, 50:52], "lmv")
        lrs = Tk(sm.a[:, 52:53], "lrs")
        zsum = Tk(sm.a[:, 53:61], "zsum")
        ssb = Tk(sm.a[:, 61:62], "ssb")
        rstdb = Tk(sm.a[:, 62:63], "rstdb")

        def dma(eng, out, in_, key, reads=(), writes=()):
            S.op(eng, lambda e: e.dma_start(out=out, in_=in_), reads, writes, dma=key)

        def mm(out, lhsT, rhs, start, stop, reads, writes):
            S.op("pe", lambda e: e.matmul(out, lhsT=lhsT, rhs=rhs, start=start, stop=stop), reads, writes)

        def tr(out, in_, reads, writes):
            S.op("pe", lambda e: e.transpose(out=out, in_=in_, identity=identb.a), list(reads) + [identb], writes)

        def act(out, in_, func, reads, writes, **kw):
            S.op("act", lambda e: e.activation(out=out, in_=in_, func=func, **kw), reads, writes)

        def tt(eng, out, in0, in1, op, reads, writes):
            S.op(eng, lambda e: e.tensor_tensor(out=out, in0=in0, in1=in1, op=op), reads, writes)

        def tsc(eng, out, in0, s1, s2, op0, op1, reads, writes):
            if s2 is None:
                S.op(eng, lambda e: e.tensor_scalar(out=out, in0=in0, scalar1=s1, scalar2=None, op0=op0), reads, writes)
            else:
                S.op(eng, lambda e: e.tensor_scalar(out=out, in0=in0, scalar1=s1, scalar2=s2, op0=op0, op1=op1), reads, writes)

        def stt(eng, out, in0, scalar, in1, op0, op1, reads, writes, accum=None):
            if accum is None:
                S.op(eng, lambda e: e.scalar_tensor_tensor(out=out, in0=in0, scalar=scalar, in1=in1, op0=op0, op1=op1), reads, writes)
            else:
                S.op(eng, lambda e: e.scalar_tensor_tensor(out=out, in0=in0, scalar=scalar, in1=in1, op0=op0, op1=op1, accum_out=accum), reads, writes)

        def cp(eng, out, in_, reads, writes):
            if eng == "act":
                S.op(eng, lambda e: e.activation(out=out, in_=in_, func=AF.Copy), reads, writes)
            else:
                S.op(eng, lambda e: e.tensor_copy(out=out, in_=in_), reads, writes)

        def rstd_from(src_tk, src_ap, dst_tk, dst_ap, mul):
            tsc("dve", dst_ap, src_ap, mul, EPS, ALU.mult, ALU.add, [src_tk], [dst_tk])
            act(dst_ap, dst_ap, AF.Sqrt, [dst_tk], [dst_tk])
            S.op("dve", lambda e: e.reciprocal(out=dst_ap, in_=dst_ap), [dst_tk], [dst_tk])

        dma("sp", identb.a, identb_d, "c0", writes=[identb])
        dma("sp", cm.a, cmisc_d, "c1", writes=[cm])
        dma("sp", cols.a, cols_d, "c2", writes=[cols])
        dma("sp", ccol.a, ccol_d, "c3", writes=[ccol])
        dma("sp", modrow.a[0:1, :], bada_d, "c4", writes=[modrow])
        dma("sp", fgb.a, fg_d.partition_broadcast(128), "c5", writes=[fgb])
        S.op("dve", lambda e: e.memset(onesb.a, 1.0), writes=[onesb])
        S.op("dve", lambda e: e.memset(state.a, 0.0), writes=[state])
        S.op("dve", lambda e: e.memset(stateb.a, 0.0), writes=[stateb])
        winb_tk = [Tk(None, "winb%d" % j) for j in range(NPA)]
        win_v = win_d.rearrange("(kt p) n -> p kt n", p=128)
        wq_v = wq_d.rearrange("(kt p) n -> p kt n", p=128)
        for j in range(NPA):
            src = win_v[:, :, j * 512:(j + 1) * 512] if j < NPIECE else wq_v[:, :, (j - NPIECE) * 512:(j - NPIECE + 1) * 512]
            dma("pool", winb_d[j].rearrange("p (k n) -> p k n", k=KT), src, "wc", writes=[winb_tk[j]])
        for j in range(NPA):
            winb_tk[j].w = (S._dsem("wc"), 16 * NPA)
        ws_v = ws_d.rearrange("(kt p) n -> p kt n", p=128)
        for kt in range(KT):
            dma("pool", Ws.a[:, kt, :], ws_v[:, kt, :], "wsg", writes=[Ws])
        puv_tk = Tk(None, "puv")
        for r in range(16):
            dma("pool", puv_d[r * 1024:(r + 1) * 1024, 0:D], pu_d[r * 1024:(r + 1) * 1024, :], "uvc")
            dma("pool", puv_d[r * 1024:(r + 1) * 1024, D:2 * D], pv_d[r * 1024:(r + 1) * 1024, :], "uvc")
        puv_tk.w = (S._dsem("uvc"), 16 * 32)
        act(cact.a, ccol.a, AF.Silu, [ccol], [cact])
        wada_v = wada_d.rearrange("(kt p) n -> p kt n", p=128)
        for j in range(48):
            wb = wab[j % 2]
            dma("sp", wb.a, wada_v[:, :, j * 128:(j + 1) * 128], "wa%d" % (j % 2), writes=[wb])
            pb = nb()
            for kt in range(KT):
                mm(pb.a[0:1, 0:128], cact.a[:, kt:kt + 1], wb.a[:, kt, :], kt == 0, kt == KT - 1, [cact, wb], [pb])
            tt("dve", modrow.a[0:1, j * 128:(j + 1) * 128], pb.a[0:1, 0:128], modrow.a[0:1, j * 128:(j + 1) * 128],
               ALU.add, [pb, modrow], [modrow])
        pb = nb()
        for si, sec in enumerate((0, 1, 3, 4)):
            for kt in range(KT):
                c0 = sec * D + kt * 128
                mm(pb.a[:, si * 8 + kt:si * 8 + kt + 1], modrow.a[0:1, c0:c0 + 128], ones_f[0:1, 0:1], True, True,
                   [modrow, cm], [pb])
        cp("dve", modcol.a, pb.a[:, 0:32], [pb], [modcol])
        stt("dve", a1col, sc1col, 1.0, g1col, ALU.add, ALU.mult, [modcol, cols], [acol])
        stt("dve", a2col, sc2col, 1.0, g2col, ALU.add, ALU.mult, [modcol, cols], [acol])
        for half in range(2):
            pb = nb()
            mm(pb.a, ones_f[0:1, :], modrow.a[0:1, 2 * D + half * 512:2 * D + (half + 1) * 512], True, True, [modrow, cm], [pb])
            cp("act", gate1b.a[:, half * 512:(half + 1) * 512], pb.a, [pb], [gate1b])
            pb = nb()
            mm(pb.a, ones_f[0:1, :], modrow.a[0:1, 5 * D + half * 512:5 * D + (half + 1) * 512], True, True, [modrow, cm], [pb])
            cp("act", gate2b.a[:, half * 512:(half + 1) * 512], pb.a, [pb], [gate2b])
        wr_v = wr_d.rearrange("(kt p) n -> p kt n", p=128)
        wo_v = wo_d.rearrange("(kt p) n -> p kt n", p=128)
        for kt in range(KT):
            st_ = stage[0]
            dma("sp", st_.a, wr_v[:, kt, :], "st0", writes=[st_])
            act(Wr.a[:, kt, :], st_.a, AF.Copy, [st_, cols], [Wr], scale=gncol[:, kt:kt + 1])
            st_ = stage[1]
            dma("sp", st_.a, wo_v[:, kt, :], "st0", writes=[st_])
            tt("dve", Wo.a[:, kt, :], st_.a, gate1b.a, ALU.mult, [st_, gate1b], [Wo])
        dma("sp", kst.a, keys_d.rearrange("k n c -> n k c"), "ks", writes=[kst])
        cp("act", kstb.a, kst.a, [kst], [kstb])
        for g in range(2):
            pb = nb()
            pbb = pb.a.bitcast(BF16).rearrange("p (k n) -> p k n", k=8)
            for j in range(8):
                tr(pbb[:, j, :], kstb.a[:, g * 8 + j, :], [kstb], [pb])
            cp("dve", keysT.a[:, g * 8:(g + 1) * 8, :], pbb, [pb], [keysT])
        dma("sp", wst.a, sguw_d.rearrange("g t s -> t g s"), "wt", writes=[wst])
        dma("sp", bsrow.a[0:1, 0:512], sgub_d, "bs", writes=[bsrow])
        cp("act", wstb.a, wst.a, [wst], [wstb])
        pb = nb()
        pbb = pb.a.bitcast(BF16).rearrange("p (k n) -> p k n", k=8)
        for g in range(4):
            tr(pbb[:, g, :], wstb.a[:, g, :], [wstb], [pb])
        tt("dve", wsT.a, pbb[:, 0:4, :], trilT.unsqueeze(1).broadcast_to([128, 4, 128]), ALU.mult, [pb, cm], [wsT])
        pA = nb()
        pB = nb()
        pA3 = pA.a.rearrange("p (k n) -> p k n", k=4)
        pB3 = pB.a.rearrange("p (k n) -> p k n", k=4)
        for g in range(4):
            mm(pA3[:, g, :], onesb.a, wsT.a[:, g, :], True, True, [onesb, wsT], [pA])
            mm(pB3[:, g, :], ones_f[0:1, :], bsrow.a[0:1, g * 128:(g + 1) * 128], True, True, [cm, bsrow], [pB])
        cp("act", bsBs.a, pB3, [pB], [bsBs])
        for ct in range(8):
            stt("dve", bsBp.a[:, ct, :], pA3[:, ct // 2, :], lnbcol[:, ct:ct + 1], bsBs.a[:, ct // 2, :], ALU.mult, ALU.add,
                [pA, cols, bsBs], [bsBp])

        total_pieces = NCH * NPA
        issued = [0]

        def ensure_w(upto):
            while issued[0] <= min(upto, total_pieces - 1):
                g_ = issued[0]
                j_ = g_ % NPA
                wb_ = wbuf[g_ % 2]
                dma("sp", wb_.a, winb_d[j_].rearrange("p (k n) -> p k n", k=KT), "wb%d" % (g_ % 2),
                    reads=[winb_tk[j_]], writes=[wb_])
                issued[0] += 1

        dma("sp", xts[0].a, x_d[0:128, :], "x0", writes=[xts[0]])
        dma("sp", cs.a, cs_d[0], "cs", writes=[cs])
        ensure_w(0)

        def norm_transpose(xt, xn_t, hT_t, acolv, shcolv):
            S.op("dve", lambda e: e.memset(ss.a, 0.0), writes=[ss])
            act(junkA.a, xt.a, AF.Square, [xt, ss], [ss, junkA], accum_out=ss.a)
            rstd_from(ss, ss.a, rstd, rstd.a, 1.0 / D)
            act(xn_t.a, xt.a, AF.Copy, [xt, rstd], [xn_t], scale=rstd.a)
            pb = nb()
            pbb = pb.a.bitcast(BF16).rearrange("p (k n) -> p k n", k=8)
            for kt in range(KT):
                tr(pbb[:, kt, :], xn_t.a[:, kt * 128:(kt + 1) * 128], [xn_t], [pb])
            for kt in range(KT):
                if kt % 2 == 0:
                    act(hT_t.a[:, kt, :], pbb[:, kt, :], AF.Identity, [pb, acol, modcol], [hT_t],
                        scale=acolv[:, kt:kt + 1], bias=shcolv[:, kt:kt + 1])
                else:
                    tsc("dve", hT_t.a[:, kt, :], pbb[:, kt, :], acolv[:, kt:kt + 1], shcolv[:, kt:kt + 1], ALU.mult, ALU.add,
                        [pb, acol, modcol], [hT_t])

        def front(i):
            xt = xts[i % 3]
            h2tok, eidx, gg = h2toks[i % 2], eidxs[i % 2], ggs[i % 2]
            norm_transpose(xt, xn, hT, a1col, sh1col)
            yield
            for j in range(NPIECE):
                gidx = i * NPA + j
                ensure_w(gidx + 1)
                wb = wbuf[gidx % 2]
                pb = nb()
                fm = j in (6, 7, 10, 11, 12, 13)
                if not fm:
                    for kt in range(KT):
                        mm(pb.a, hT.a[:, kt, :], wb.a[:, kt, :], kt == 0, kt == KT - 1, [hT, wb], [pb])
                else:
                    p3 = pb.a.rearrange("p (k n) -> p k n", k=4)
                    for n in range(4):
                        for kt in range(KT):
                            mm(p3[:, n, :], wb.a[:, kt, n * 128:(n + 1) * 128], hT.a[:, kt, :], kt == 0, kt == KT - 1,
                               [hT, wb], [pb])
                if j < 2:
                    act(qk.a[:, j * 4:(j + 1) * 4, :], pb.a.rearrange("p (h n) -> p h n", h=4), AF.Copy, [pb], [qk])
                elif j < 4:
                    act(vv.a[:, (j - 2) * 512:(j - 1) * 512], pb.a, AF.Copy, [pb], [vv])
                elif j < 6:
                    act(sg.a[:, (j - 4) * 512:(j - 3) * 512], pb.a, AF.Silu, [pb], [sg])
                elif j < 8:
                    act(guT.a[:, (j - 6) * 4:(j - 5) * 4, :], p3, AF.Gelu, [pb], [guT])
                elif j < 10:
                    act(gsv.a[:, (j - 8) * 512:(j - 7) * 512], pb.a, AF.Gelu, [pb], [gsv])
                elif j < 12:
                    act(sigA.a[:, (j - 10) * 4:(j - 9) * 4, :], p3, AF.Sigmoid, [pb], [sigA])
                else:
                    act(sigB.a[:, (j - 12) * 4:(j - 11) * 4, :], p3, AF.Sigmoid, [pb], [sigB])
                if j == 1:
                    c2 = cs.a[:, 0:128].unsqueeze(1).broadcast_to([128, 8, 128])
                    sn = cs.a[:, 128:192].unsqueeze(1).broadcast_to([128, 8, 64])
                    sp_ = cs.a[:, 192:256].unsqueeze(1).broadcast_to([128, 8, 64])
                    tt("dve", rA.a, qk.a, c2, ALU.mult, [qk, cs], [rA])
                    tt("dve", rB.a[:, :, 0:64], qk.a[:, :, 64:128], sn, ALU.mult, [qk, cs], [rB])
                    tt("dve", rB.a[:, :, 64:128], qk.a[:, :, 0:64], sp_, ALU.mult, [qk, cs], [rB])
                    tt("dve", rA.a, rA.a, rB.a, ALU.add, [rA, rB], [rA])
                    yield
                    for h in range(4):
                        act(qd.a[:, h, :], rA.a[:, h, :], AF.Copy, [rA, cm], [qd], scale=dq[:, h:h + 1])
                        act(kp.a[:, h, :], rA.a[:, 4 + h, :], AF.Copy, [rA, cm], [kp], scale=dk[:, h:h + 1])
                    pt_ = nb()
                    ptb = pt_.a.bitcast(BF16).rearrange("p (k n) -> p k n", k=8)
                    for h in range(4):
                        tr(ptb[:, h, :], qd.a[:, h, :], [qd], [pt_])
                        tr(ptb[:, 4 + h, :], kp.a[:, h, :], [kp], [pt_])
                    cp("act", qkT.a, ptb, [pt_], [qkT])
                    psc = nb()
                    ps3 = psc.a.rearrange("p (h n) -> p h n", h=4)
                    for h in range(4):
                        mm(ps3[:, h, :], qkT.a[:, 4 + h, :], qkT.a[:, h, :], True, True, [qkT], [psc])
                    tt("dve", sTm.a, ps3, trilT.unsqueeze(1).broadcast_to([128, 4, 128]), ALU.mult, [psc, cm], [sTm])
                yield
            pY = nb(2)
            pD = nb(2)
            for h in range(4):
                o = pY[h // 2].a[:, (h % 2) * 256:(h % 2 + 1) * 256]
                mm(o, sTm.a[:, h, :], vv.a[:, h * 256:(h + 1) * 256], True, False, [sTm, vv], [pY[h // 2]])
                mm(o, qkT.a[:, h, :], stateb.a[:, h * 256:(h + 1) * 256], False, True, [qkT, stateb], [pY[h // 2]])
            for h in range(4):
                o = pD[h // 2].a[:, (h % 2) * 256:(h % 2 + 1) * 256]
                mm(o, kp.a[:, h, :], vv.a[:, h * 256:(h + 1) * 256], True, True, [kp, vv], [pD[h // 2]])
            tmpS_a = qk.a.rearrange("p h n -> p (h n)")
            for hh in range(2):
                tt("dve", tmpS_a[:, hh * 512:(hh + 1) * 512], pD[hh].a, state.a[:, hh * 512:(hh + 1) * 512], ALU.add,
                   [pD[hh], state], [qk])
            for h in range(4):
                act(state.a[:, h * 256:(h + 1) * 256], tmpS_a[:, h * 256:(h + 1) * 256], AF.Copy, [qk], [state],
                    scale=float(gC[h]))
            cp("act", stateb.a, state.a, [state], [stateb])
            for h in range(4):
                src = pY[h // 2].a[:, (h % 2) * 256:(h % 2 + 1) * 256]
                S.op("dve", lambda e, src=src, h=h: e.bn_stats(out=bst.a[:, h, :], in_=src), [pY[h // 2]], [bst])
            for h in range(4):
                S.op("dve", lambda e, h=h: e.bn_aggr(out=bmv.a[:, h, :], in_=bst.a[:, h, :]), [bst], [bmv])
            rstd_from(bmv, bmv.a[:, :, 1], brs, brs.a, 1.0)
            yn = rA.a.rearrange("p h n -> p (h n)")
            for h in range(4):
                src = pY[h // 2].a[:, (h % 2) * 256:(h % 2 + 1) * 256]
                tsc("dve", yn[:, h * 256:(h + 1) * 256], src, bmv.a[:, h, 0:1], brs.a[:, h:h + 1], ALU.subtract, ALU.mult,
                    [pY[h // 2], bmv, brs], [rA])
            tt("dve", zz.a, sg.a, yn, ALU.mult, [sg, rA], [zz])
            yield
            pt_ = nb()
            ptb = pt_.a.bitcast(BF16).rearrange("p (k n) -> p k n", k=8)
            for kt in range(KT):
                tr(ptb[:, kt, :], zz.a[:, kt * 128:(kt + 1) * 128], [zz], [pt_])
            cp("act", zT.a, ptb, [pt_], [zT])
            pA_ = nb(2)
            for n in range(8):
                o = pA_[n // 4].a.rearrange("p (k n) -> p k n", k=4)[:, n % 4, :]
                for kt in range(KT):
                    mm(o, Wr.a[:, kt, n * 128:(n + 1) * 128], zT.a[:, kt, :], kt == 0, kt == KT - 1, [Wr, zT], [pA_[n // 4]])
                if n == 3:
                    yield
            m1 = gsv.a.rearrange("p (k n) -> p k n", k=KT)
            m2 = sg.a.rearrange("p (k n) -> p k n", k=KT)
            for c in range(2):
                S.op("dve", lambda e, c=c: e.bn_stats(out=lst.a[:, c * 6:(c + 1) * 6], in_=gsv.a[:, c * 512:(c + 1) * 512]),
                     [gsv], [lst])
            S.op("dve", lambda e: e.bn_aggr(out=lmv.a, in_=lst.a), [lst], [lmv])
            rstd_from(lmv, lmv.a[:, 1:2], lrs, lrs.a, 1.0)
            tsc("dve", vhat.a, gsv.a, lmv.a[:, 0:1], lrs.a, ALU.subtract, ALU.mult, [gsv, lmv, lrs], [vhat])
            yield
            for hh in range(2):
                tt("dve", m1[:, hh * 4:(hh + 1) * 4, :], pA_[hh].a.rearrange("p (k n) -> p k n", k=4),
                   sigA.a[:, hh * 4:(hh + 1) * 4, :], ALU.mult, [pA_[hh], sigA], [gsv])
            pM = nb(2)
            for ct in range(8):
                o = pM[ct // 4].a.rearrange("p (k n) -> p k n", k=4)[:, ct % 4, :]
                mm(o, vhat.a[:, ct * 128:(ct + 1) * 128], wsT.a[:, ct // 2, :], True, True, [vhat, wsT], [pM[ct // 4]])
            t1 = rB.a
            for ct in range(8):
                src = pM[ct // 4].a.rearrange("p (k n) -> p k n", k=4)[:, ct % 4, :]
                stt("dve", t1[:, ct, :], src, lngcol[:, ct:ct + 1], bsBp.a[:, ct, :], ALU.mult, ALU.add,
                    [pM[ct // 4], cols, bsBp], [rB])
            tt("dve", ysT.a, guT.a, t1, ALU.mult, [guT, rB], [ysT])
            yield
            pB_ = nb(2)
            for n in range(8):
                o = pB_[n // 4].a.rearrange("p (k n) -> p k n", k=4)[:, n % 4, :]
                for kt in range(KT):
                    mm(o, Ws.a[:, kt, n * 128:(n + 1) * 128], ysT.a[:, kt, :], kt == 0, kt == KT - 1, [Ws, ysT], [pB_[n // 4]])
                if n == 3:
                    yield
            for hh in range(2):
                tt("dve", m2[:, hh * 4:(hh + 1) * 4, :], pB_[hh].a.rearrange("p (k n) -> p k n", k=4),
                   sigB.a[:, hh * 4:(hh + 1) * 4, :], ALU.mult, [pB_[hh], sigB], [sg])
            tt("dve", mergedT.a, m1, m2, ALU.add, [gsv, sg], [mergedT])
            yield
            pO = nb(2)
            for hh in range(2):
                for kt in range(KT):
                    mm(pO[hh].a, mergedT.a[:, kt, :], Wo.a[:, kt, hh * 512:(hh + 1) * 512], kt == 0, kt == KT - 1,
                       [mergedT, Wo], [pO[hh]])
            for hh in range(2):
                tt("dve", xt.a[:, hh * 512:(hh + 1) * 512], pO[hh].a, xt.a[:, hh * 512:(hh + 1) * 512], ALU.add,
                   [pO[hh], xt], [xt])
            yield
            norm_transpose(xt, xn2, h2T, a2col, sh2col)
            pt_ = nb()
            ptb = pt_.a.bitcast(BF16)
            for kt in range(KT):
                tr(ptb[:, kt * 128:(kt + 1) * 128], h2T.a[:, kt, :], [h2T], [pt_])
            cp("act", h2tok.a, ptb, [pt_], [h2tok])
            yield
            for p_ in range(4):
                gidx = i * NPA + NPIECE + p_
                ensure_w(gidx + 1)
                wb = wbuf[gidx % 2]
                pb = nb()
                p3 = pb.a.rearrange("p (k n) -> p k n", k=4)
                for n in range(4):
                    for kt in range(KT):
                        mm(p3[:, n, :], wb.a[:, kt, n * 128:(n + 1) * 128], h2T.a[:, kt, :], kt == 0, kt == KT - 1,
                           [h2T, wb], [pb])
                cp("act", qT.a[:, p_ * 4:(p_ + 1) * 4, :], p3, [pb], [qT])
                yield
            for g in range(4):
                pb = nb()
                p3 = pb.a.rearrange("p (k n) -> p k n", k=4)
                for n in range(4):
                    hp = g * 4 + n
                    mm(p3[:, n, :], qT.a[:, hp, :], keysT.a[:, hp, :], True, True, [qT, keysT], [pb])
                cp("act", s3[:, g * 4:(g + 1) * 4, :], p3, [pb], s_hp[g * 4:(g + 1) * 4])
            yield
            for rnd in range(2):
                lo, hi = rnd * 8, rnd * 8 + 8
                for hp in range(16):
                    S.op("dve", lambda e, hp=hp, lo=lo, hi=hi: e.max(out=v1.a[:, hp, lo:hi], in_=s3[:, hp, :]),
                         [s_hp[hp]], [v1_hp[hp]])
                    if hp % 8 == 7:
                        yield
                for hp in range(16):
                    S.op("dve", lambda e, hp=hp, lo=lo, hi=hi: e.max_index(out=i1.a[:, hp, lo:hi], in_max=v1.a[:, hp, lo:hi],
                                                                          in_values=s3[:, hp, :]),
                         [s_hp[hp], v1_hp[hp]], [i1_hp[hp]])
                    if hp % 8 == 7:
                        yield
                if rnd == 0:
                    for hp in range(16):
                        S.op("dve", lambda e, hp=hp: e.match_replace(out=s3[:, hp, :], in_to_replace=v1.a[:, hp, 0:8],
                                                                     in_values=s3[:, hp, :], imm_value=-1e30),
                             [v1_hp[hp]], [s_hp[hp]])
                        if hp % 8 == 7:
                            yield
            v14 = v1.a.rearrange("p (h two) k -> p h two k", two=2)
            c4 = W8.a.rearrange("p (h a b) -> p h a b", h=8, a=16)
            tt("dve", c4, v14[:, :, 0, :].unsqueeze(3).broadcast_to([128, 8, 16, 16]),
               v14[:, :, 1, :].unsqueeze(2).broadcast_to([128, 8, 16, 16]), ALU.add, v1_hp, c_h)
            yield
            for rnd in range(2):
                lo, hi = rnd * 8, rnd * 8 + 8
                for h in range(8):
                    S.op("dve", lambda e, h=h, lo=lo, hi=hi: e.max(out=tsv.a[:, h, lo:hi], in_=c3[:, h, :]), [c_h[h]], [ts_h[h]])
                for h in range(8):
                    S.op("dve", lambda e, h=h, lo=lo, hi=hi: e.max_index(out=tiv.a[:, h, lo:hi], in_max=tsv.a[:, h, lo:hi],
                                                                        in_values=c3[:, h, :]),
                         [c_h[h], ts_h[h]], [ti_h[h]])
                yield
                if rnd == 0:
                    for h in range(8):
                        S.op("dve", lambda e, h=h: e.match_replace(out=c3[:, h, :], in_to_replace=tsv.a[:, h, 0:8],
                                                                   in_values=c3[:, h, :], imm_value=-1e30),
                             [ts_h[h]], [c_h[h]])
                yield
            ti2 = tiv.a.rearrange("p h k -> p (h k)")
            S.op("dve", lambda e: e.tensor_single_scalar(out=tab.a[:, 0:128], in_=ti2, scalar=4, op=ALU.logical_shift_right),
                 ti_h, [tab])
            S.op("dve", lambda e: e.tensor_single_scalar(out=tab.a[:, 128:256], in_=ti2, scalar=15, op=ALU.bitwise_and),
                 ti_h, [tab])
            cp("dve", abf.a, tab.a, [tab], [abf])
            cp("dve", i1f.a, i1.a, i1_hp, [i1f])
            i1f4 = i1f.a.rearrange("p (h two) k -> p h two k", two=2)
            io4 = iota16.unsqueeze(1).unsqueeze(1).broadcast_to([128, 8, 16, 16])
            for p_ in range(2):
                sel = abf.a[:, p_ * 128:(p_ + 1) * 128].rearrange("p (h k) -> p h k", h=8)
                tt("dve", oh.a, sel.unsqueeze(3).broadcast_to([128, 8, 16, 16]), io4, ALU.is_equal, [abf, cm], [oh])
                yield
                tt("dve", oh.a, oh.a, i1f4[:, :, p_, :].unsqueeze(2).broadcast_to([128, 8, 16, 16]), ALU.mult, [oh, i1f], [oh])
                yield
                S.op("dve", lambda e, p_=p_: e.tensor_reduce(out=k12.a[:, p_ * 128:(p_ + 1) * 128],
                                                            in_=oh.a.rearrange("p h k j -> p (h k) j"), axis=AX.X, op=ALU.add),
                     [oh], [k12])
                yield
            stt("dve", ef.a, k12.a[:, 0:128], 128.0, k12.a[:, 128:256], ALU.mult, ALU.add, [k12], [ef])
            tsc("dve", ef.a, ef.a, 0.0, 16383.0, ALU.max, ALU.min, [ef], [ef])
            cp("dve", eidx.a, ef.a, [ef], [eidx])
            tt("dve", gex.a, tsv.a, tsv.a[:, :, 0:1].broadcast_to([128, 8, 16]), ALU.subtract, ts_h, [gex])
            act(gex.a, gex.a, AF.Exp, [gex], [gex])
            S.op("dve", lambda e: e.tensor_reduce(out=zsum.a, in_=gex.a, axis=AX.X, op=ALU.add), [gex], [zsum])
            S.op("dve", lambda e: e.reciprocal(out=zsum.a, in_=zsum.a), [zsum], [zsum])
            tt("dve", gg.a, gex.a, zsum.a.unsqueeze(2).broadcast_to([128, 8, 16]), ALU.mult, [gex, zsum], [gg])
            yield

        gi = [0]
        def back(i):
            pP = banks[4:6] if i % 2 == 0 else banks[6:8]
            xt = xts[i % 3]
            h2tok, eidx, gg = h2toks[i % 2], eidxs[i % 2], ggs[i % 2]
            ggf = gg.a.rearrange("p h k -> p (h k)")
            S.op("dve", lambda e: e.memset(pre.a, 0.0), writes=pre_c)
            G = 2
            NG = 128 // G
            hk_slot = {}

            def stageA(g):
                for hk in range(g * G, (g + 1) * G):
                    sl = slots[gi[0] % NSLOT]
                    key = "g%d" % (gi[0] % NSLOT)
                    jd = junkDs[gi[0] % 2]
                    gi[0] += 1
                    hk_slot[hk] = sl
                    S.op("pool", lambda e, sl=sl, hk=hk: e.indirect_dma_start(
                        out=sl.a, out_offset=None, in_=puv_d,
                        in_offset=bass.IndirectOffsetOnAxis(ap=eidx.a[:, hk:hk + 1], axis=0)),
                        [eidx, puv_tk], [sl], dma=key)
                    stt("dve", jd.a, sl.a[:, 0:D], 1.0, h2tok.a, ALU.mult, ALU.mult, [sl, h2tok, pre_c[hk]],
                        [pre_c[hk], jd], accum=pre.a[:, hk:hk + 1])

            def stageC(g):
                c0, c1 = g * G, (g + 1) * G
                act(glt.a[:, c0:c1], pre.a[:, c0:c1], AF.Gelu, pre_c[c0:c1], gl_c[c0:c1])
                for hk in range(c0, c1):
                    act(glg.a[:, hk:hk + 1], glt.a[:, hk:hk + 1], AF.Copy, [gl_c[hk], gg], [glg_c[hk]], scale=ggf[:, hk:hk + 1])
                for hk in range(c0, c1):
                    sl = hk_slot.pop(hk)
                    dgt = dg[hk % 4]
                    act(dgt.a, identf, AF.Copy, [cm, glg_c[hk]], [dgt], scale=glg.a[:, hk:hk + 1])
                    for hh in range(2):
                        mm(pP[hh].a, dgt.a, sl.a[:, D + hh * 512:D + (hh + 1) * 512], hk == 0, hk == 127, [dgt, sl], [pP[hh]])

            for step in range(NG + 1):
                if step < NG:
                    stageA(step)
                if step >= 1:
                    stageC(step - 1)
                yield
            yield "T"
            for hh in range(2):
                tt("dve", rtmp.a, pP[hh].a, gate2b.a[:, hh * 512:(hh + 1) * 512], ALU.mult, [pP[hh], gate2b], [rtmp])
                tt("dve", xt.a[:, hh * 512:(hh + 1) * 512], rtmp.a, xt.a[:, hh * 512:(hh + 1) * 512], ALU.add, [rtmp, xt], [xt])
            S.op("dve", lambda e: e.memset(ssb.a, 0.0), writes=[ssb])
            act(junkA.a, xt.a, AF.Square, [xt, ssb], [ssb, junkA], accum_out=ssb.a)
            rstd_from(ssb, ssb.a, rstdb, rstdb.a, 1.0 / D)
            stt("dve", xt.a, xt.a, rstdb.a, fgb.a, ALU.mult, ALU.mult, [xt, rstdb, fgb], [xt])
            S.op("act", lambda e, i=i, xt=xt: e.dma_start(out=out_d[i * 128:(i + 1) * 128, :], in_=xt.a), [xt], [], dma="out%d" % (i % 3))
            yield

        def prefetch_x(i):
            if i < NCH:
                dma("sp", xts[i % 3].a, x_d[i * 128:(i + 1) * 128, :], "x%d" % (i % 3), writes=[xts[i % 3]])

        def drain(g):
            for _ in g:
                pass

        drain(front(0))
        FRAC = 0.9
        RUNMAX = 4
        DVEW = 2.5
        KDEF = 4
        nbk = 128 // 2 + 1
        pending = None
        for i in range(NCH):
            b = back(i)
            bd = 0
            for _ in range(KDEF):
                next(b)
                bd += 1
            if pending is not None:
                drain(pending)
            bdone = False

            def step_back():
                nonlocal_state = None
                return next(b)

            if i + 1 < NCH:
                prefetch_x(i + 1)
                dma("sp", cs.a, cs_d[i + 1], "cs", writes=[cs])
                S.rec = []
                drain(front(i + 1))
                ops, S.rec = S.rec, None
                n_ops = len(ops)
                wts = [DVEW if o[0] == "dve" else 1.0 for o in ops]
                wtot = sum(wts)
                wacc = 0.0
                run = 0
                for k, (eng, fn, rd, wr, dm) in enumerate(ops):
                    S.op(eng, fn, rd, wr, dm)
                    run += 1
                    wacc += wts[k]
                    if k + 1 < n_ops and ops[k + 1][0] == eng and run < RUNMAX:
                        continue
                    run = 0
                    while not bdone and bd < KDEF + (wacc / wtot) * FRAC * (nbk - KDEF):
                        if next(b) == "T":
                            bdone = True
                        else:
                            bd += 1
            while not bdone:
                if next(b) == "T":
                    bdone = True
            pending = b
        drain(pending)

        fin = []
        for kk in ("out0", "out1", "out2"):
            if kk in S.dsem:
                fin.append((S.dsem[kk], S.cnt[S.dsem[kk]]))
        S.finish(fin)
        S.run()
    return nc, S.nins


def _consts(NCH):
    H, C = 4, 128
    gamma = 1.0 - 2.0 ** (-5.0 - np.arange(H, dtype=np.float64))
    i = np.arange(C, dtype=np.float64)
    cm = np.zeros((128, 408), np.float32)
    cm[:, 0:128] = (np.arange(128)[None, :] >= np.arange(128)[:, None]).astype(np.float32)
    cm[:, 128:132] = (gamma[None, :] ** (i[:, None] + 1.0)).astype(np.float32)
    cm[:, 132:136] = (gamma[None, :] ** (-(i[:, None] + 1.0)) * (128.0 ** -0.5)).astype(np.float32)
    cm[:, 136:152] = np.arange(16, dtype=np.float32)[None, :]
    cm[:, 152:280] = 1.0
    cm[:, 280:408] = np.eye(128, dtype=np.float32)
    gC = (gamma ** C).astype(np.float64)
    half = 64
    inv = (np.float32(10000.0) ** (-np.arange(half, dtype=np.float32) * np.float32(2.0) / np.float32(128))).astype(np.float32)
    pos = np.arange(NCH * 128, dtype=np.float32)
    ang = (pos[:, None] * inv[None, :]).astype(np.float32)
    cos, sin = np.cos(ang).astype(np.float32), np.sin(ang).astype(np.float32)
    cs = np.concatenate([cos, cos, -sin, sin], axis=1).reshape(NCH, 128, 256).astype(np.float32)
    identb = np.eye(128, dtype=np.float32).astype(ml_dtypes.bfloat16)
    return cm, gC, cs, identb


def _col(v):
    return np.ascontiguousarray(np.asarray(v, np.float32).reshape(8, 128).T)


_CACHE = {}


def run_cores(inputs, NCH, cores):
    f = lambda k: np.asarray(inputs[k], np.float32)
    cm, gC, cs, identb = _consts(NCH)
    key = NCH
    if key not in _CACHE:
        _CACHE[key] = build_program(NCH, gC)
    nc, nins = _CACHE[key]
    cols = np.concatenate([_col(f("norm1_g")[0]), _col(f("norm2_g")[0]), _col(f("ret_gn_g")[0]),
                           _col(f("sgu_ln_g")[0]), _col(f("sgu_ln_b")[0])], axis=1)
    shared = {
        "w_ada": np.ascontiguousarray(f("w_ada")[0]), "b_ada": np.ascontiguousarray(f("b_ada")[0].reshape(1, -1)),
        "cols": np.ascontiguousarray(cols), "w_in": np.ascontiguousarray(f("w_in")[0]),
        "w_ret_out": np.ascontiguousarray(f("w_ret_out")[0]), "w_sgu_out": np.ascontiguousarray(f("w_sgu_out")[0]),
        "w_out": np.ascontiguousarray(f("w_out")[0]), "w_q": np.ascontiguousarray(f("peer_w_q")[0]),
        "keys": np.ascontiguousarray(f("peer_sub_keys")[0].reshape(16, 128, 128)),
        "sgu_w": np.ascontiguousarray(f("sgu_w")[0]), "sgu_b": np.ascontiguousarray(f("sgu_b")[0].reshape(1, 512)),
        "final_g": np.ascontiguousarray(f("final_g").reshape(1, -1)),
        "peer_u": np.ascontiguousarray(f("peer_u")[0]), "peer_v": np.ascontiguousarray(f("peer_v")[0]),
        "identb": identb, "cmisc": cm, "cs": cs,
    }
    x, c = f("x"), f("c")
    in_maps = []
    for b in cores:
        m = dict(shared)
        m["x"] = np.ascontiguousarray(x[b, :NCH * 128])
        m["ccol"] = _col(c[b])
        in_maps.append(m)
    res = run_bass_kernel_spmd(nc, in_maps, core_ids=list(range(len(cores))))
    return np.stack([np.asarray(r["out"], np.float32) for r in res.results], axis=0)


def kernel(**inputs):
    return run_cores(inputs, 32, list(range(8))).astype(np.float32)
```

```python
import numpy as np
from contextlib import ExitStack
import ml_dtypes
import concourse.bass as bass
import concourse.mybir as mybir
from concourse.bass_utils import run_bass_kernel_spmd

F32 = mybir.dt.float32
BF16 = mybir.dt.bfloat16
I32 = mybir.dt.int32
U32 = mybir.dt.uint32
AF = mybir.ActivationFunctionType
ALU = mybir.AluOpType
AX = mybir.AxisListType

D = 1024
KT = 8
NPIECE = 14
NPA = 18
EPS = 1e-6
NSLOT = 12
ARENA = 41 * 1024
INTERLEAVE = True


class Tk:
    __slots__ = ("a", "w", "r", "al", "name", "lay", "off", "size")

    def __init__(self, a, name=""):
        self.a = a
        self.w = None
        self.r = {}
        self.al = []
        self.name = name
        self.lay = None
        self.off = 0
        self.size = 0


class Sched:
    ENG = ("pe", "act", "dve", "pool", "sp")

    def __init__(self, nc, es):
        self.nc = nc
        self.es = es
        self.q = {e: [] for e in self.ENG}
        self.sem = {e: es.enter_context(nc.semaphore("s_" + e)) for e in self.ENG}
        self.cnt = {}
        self.known = {e: {} for e in self.ENG}
        self.dsem = {}
        self.arena = None
        self.lay_off = {}
        self.carved = []
        self.nins = {e: 0 for e in self.ENG}
        self.rec = None

    def tile(self, name, shape, dt):
        t = self.es.enter_context(self.nc.sbuf_tensor(name, list(shape), dt))
        return Tk(t[:], name)

    def psum(self, name, shape, dt):
        t = self.es.enter_context(self.nc.psum_tensor(name, list(shape), dt))
        return Tk(t[:], name)

    def carve(self, lay, name, nbytes, dt, pattern=None, **kw):
        if self.arena is None:
            self.arena = self.es.enter_context(self.nc.sbuf_tensor("arena", [128, ARENA // 4], F32))
        off = self.lay_off.get(lay, 0)
        nbytes = (nbytes + 31) // 32 * 32
        assert off + nbytes <= ARENA, (lay, name, off, nbytes)
        self.lay_off[lay] = off + nbytes
        a = self.arena[:, off // 4:(off + nbytes) // 4]
        if dt != F32:
            a = a.bitcast(dt)
        if pattern is not None:
            a = a.rearrange(pattern, **kw)
        t = Tk(a, name)
        t.lay, t.off, t.size = lay, off, nbytes
        for u in self.carved:
            if u.lay != lay and u.off < off + nbytes and off < u.off + u.size:
                u.al.append(t)
                t.al.append(u)
        self.carved.append(t)
        return t

    def sub(self, parent, a, name=""):
        t = Tk(a, name or parent.name)
        t.al = list(parent.al)
        for u in parent.al:
            u.al.append(t)
        return t

    @staticmethod
    def alias(xs, ys):
        for x in xs:
            for y in ys:
                x.al.append(y)
                y.al.append(x)

    def _dsem(self, key):
        if key not in self.dsem:
            self.dsem[key] = self.es.enter_context(self.nc.semaphore("d_" + key))
        return self.dsem[key]

    def op(self, eng, fn, reads=(), writes=(), dma=None):
        if self.rec is not None:
            self.rec.append((eng, fn, list(reads), list(writes), dma))
            return None
        deps = {}

        def add(d):
            if d is not None and deps.get(d[0], 0) < d[1]:
                deps[d[0]] = d[1]

        for t in reads:
            add(t.w)
            for u in t.al:
                add(u.w)
        for t in writes:
            add(t.w)
            for s, v in t.r.items():
                add((s, v))
            for u in t.al:
                add(u.w)
                for s, v in u.r.items():
                    add((s, v))
        own = self.sem[eng]
        waits = []
        kn = self.known[eng]
        for s, v in deps.items():
            if eng == "pe" and s is own:
                continue
            if kn.get(s, 0) >= v:
                continue
            kn[s] = v
            waits.append((s, v))
        if dma is None:
            s, inc = own, 1
        else:
            s, inc = self._dsem(dma), 16
        val = self.cnt.get(s, 0) + inc
        self.cnt[s] = val
        self.nins[eng] += 1 + len(waits)

        def emit(e, waits=waits, fn=fn, s=s, inc=inc):
            for ws, wv in waits:
                e.wait_ge(ws, wv)
            fn(e).then_inc(s, inc)

        self.q[eng].append(emit)
        for t in writes:
            t.w = (s, val)
            t.r = {}
        for t in reads:
            if t.r.get(s, 0) < val:
                t.r[s] = val
        return (s, val)

    def finish(self, final_deps):
        def emit(e):
            for s, v in final_deps:
                e.wait_ge(s, v)
        self.q["sp"].append(emit)

    def run(self):
        with self.nc.Block() as block:
            @block.tensor
            def _(e):
                for f in self.q["pe"]:
                    f(e)

            @block.scalar
            def _(e):
                for f in self.q["act"]:
                    f(e)

            @block.vector
            def _(e):
                for f in self.q["dve"]:
                    f(e)

            @block.gpsimd
            def _(e):
                for f in self.q["pool"]:
                    f(e)

            @block.sync
            def _(e):
                for f in self.q["sp"]:
                    f(e)


def build_program(NCH, gC):
    nc = bass.Bass("TRN2", target_bir_lowering=False)

    def din(name, shape, dt=F32):
        return nc.dram_tensor(name, list(shape), dt, kind="ExternalInput").ap()

    T = NCH * 128
    x_d = din("x", [T, D])
    out_d = nc.dram_tensor("out", [T, D], F32, kind="ExternalOutput").ap()
    ccol_d = din("ccol", [128, 8])
    wada_d = din("w_ada", [D, 6 * D])
    bada_d = din("b_ada", [1, 6 * D])
    cols_d = din("cols", [128, 40])
    win_d = din("w_in", [D, 7168])
    wr_d = din("w_ret_out", [D, D])
    ws_d = din("w_sgu_out", [D, D])
    wo_d = din("w_out", [D, D])
    wq_d = din("w_q", [D, 2048])
    keys_d = din("keys", [16, 128, 128])
    sguw_d = din("sgu_w", [4, 128, 128])
    sgub_d = din("sgu_b", [1, 512])
    fg_d = din("final_g", [1, D])
    pu_d = din("peer_u", [16384, D])
    pv_d = din("peer_v", [16384, D])
    identb_d = din("identb", [128, 128], BF16)
    cmisc_d = din("cmisc", [128, 408])
    cs_d = din("cs", [NCH, 128, 256])
    winb_d = nc.dram_tensor("winb", [NPA, 128, KT * 512], BF16).ap()
    puv_d = nc.dram_tensor("puv", [16384, 2 * D], BF16).ap()

    with ExitStack() as es:
        S = Sched(nc, es)
        Wr = S.tile("Wr", [128, KT, D], BF16)
        Ws = S.tile("Ws", [128, KT, D], BF16)
        Wo = S.tile("Wo", [128, KT, D], BF16)
        wbuf = [S.tile("wbuf%d" % i, [128, KT, 512], BF16) for i in range(2)]
        keysT = S.tile("keysT", [128, 16, 128], BF16)
        wsT = S.tile("wsT", [128, 4, 128], BF16)
        bsBp = S.tile("bsBp", [128, 8, 128], F32)
        gate2b = S.tile("gate2b", [128, D], F32)
        fgb = S.tile("fgb", [128, D], F32)
        state = S.tile("state", [128, D], F32)
        stateb = S.tile("stateb", [128, D], BF16)
        identb = S.tile("identb_s", [128, 128], BF16)
        onesb = S.tile("onesb", [128, 128], BF16)
        cm = S.tile("cmisc_s", [128, 408], F32)
        cols = S.tile("cols_s", [128, 40], F32)
        modcol = S.tile("modcol", [128, 32], F32)
        acol = S.tile("acol", [128, 16], F32)
        xts = [S.tile("xt%d" % i, [128, D], F32) for i in range(3)]
        junkA = S.tile("junkA", [128, D], BF16)
        junkDs = [S.tile("junkD0", [128, D], BF16)] * 2
        sm = S.tile("smalls", [128, 64], F32)
        h2toks = [S.tile("h2tok%d" % i, [128, D], BF16) for i in range(2)]
        eidxs = [S.tile("eidx%d" % i, [128, 128], I32) for i in range(2)]
        ggs = [S.tile("gg%d" % i, [128, 8, 16], F32) for i in range(2)]
        pre = S.tile("pre", [128, 128], F32)
        glt = S.tile("glt", [128, 128], F32)
        glg = S.tile("glg", [128, 128], F32)
        dg = [S.tile("dg%d" % i, [128, 128], BF16) for i in range(4)]
        rtmp = S.tile("rtmp", [128, 512], F32)
        slots = [S.tile("slot%d" % i, [128, 2 * D], BF16) for i in range(NSLOT)]
        banks = [S.psum("bank%d" % i, [128, 512], F32) for i in range(8)]
        bank_rr = [0]

        def nb(n=1):
            r = []
            for _ in range(n):
                r.append(banks[bank_rr[0] % 4])
                bank_rr[0] += 1
            return r if n > 1 else r[0]

        trilT = cm.a[:, 0:128]
        dq = cm.a[:, 128:132]
        dk = cm.a[:, 132:136]
        iota16 = cm.a[:, 136:152]
        ones_f = cm.a[:, 152:280]
        identf = cm.a[:, 280:408]
        g1col, g2col = cols.a[:, 0:8], cols.a[:, 8:16]
        gncol, lngcol, lnbcol = cols.a[:, 16:24], cols.a[:, 24:32], cols.a[:, 32:40]
        sh1col, sc1col = modcol.a[:, 0:8], modcol.a[:, 8:16]
        sh2col, sc2col = modcol.a[:, 16:24], modcol.a[:, 24:32]
        a1col, a2col = acol.a[:, 0:8], acol.a[:, 8:16]

        ccol = S.carve("P1", "ccol", 32, F32)
        cact = S.carve("P1", "cact", 32, F32)
        wab = [S.carve("P1", "wab%d" % i, 4096, F32, "p (k n) -> p k n", k=KT) for i in range(2)]
        modrow = S.carve("P1", "modrow", 24576, F32)
        gate1b = S.carve("P1", "gate1b", 4096, F32)
        stage = [S.carve("P1", "stage0", 4096, F32)] * 2
        kst = S.carve("P2", "kst", 8192, F32, "p (k n) -> p k n", k=16)
        kstb = S.carve("P2", "kstb", 4096, BF16, "p (k n) -> p k n", k=16)
        wst = S.carve("P2", "wst", 2048, F32, "p (k n) -> p k n", k=4)
        wstb = S.carve("P2", "wstb", 1024, BF16, "p (k n) -> p k n", k=4)
        bsBs = S.carve("P2", "bsBs", 2048, F32, "p (k n) -> p k n", k=4)
        bsrow = S.carve("P2", "bsrow", 2048, F32)
        xn = S.carve("M", "xn", 2048, BF16)
        hT = S.carve("M", "hT", 2048, BF16, "p (k n) -> p k n", k=KT)
        cs = S.carve("M", "cs", 1024, F32)
        qk = S.carve("M", "qk", 4096, F32, "p (h n) -> p h n", h=8)
        rA = S.carve("M", "rA", 4096, F32, "p (h n) -> p h n", h=8)
        rB = S.carve("M", "rB", 4096, F32, "p (h n) -> p h n", h=8)
        qd = S.carve("M", "qd", 1024, BF16, "p (h n) -> p h n", h=4)
        kp = S.carve("M", "kp", 1024, BF16, "p (h n) -> p h n", h=4)
        qkT = S.carve("M", "qkT", 2048, BF16, "p (h n) -> p h n", h=8)
        vv = S.carve("M", "v", 2048, BF16)
        sg = S.carve("M", "sg", 4096, F32)
        guT = S.carve("M", "guT", 2048, BF16, "p (k n) -> p k n", k=KT)
        gsv = S.carve("M", "gsv", 4096, F32)
        vhat = S.carve("M", "vhat", 2048, BF16)
        sigA = S.carve("M", "sigA", 2048, BF16, "p (k n) -> p k n", k=KT)
        sigB = S.carve("M", "sigB", 2048, BF16, "p (k n) -> p k n", k=KT)
        sTm = S.carve("M", "sTm", 1024, BF16, "p (h n) -> p h n", h=4)
        zz = xn
        zT = hT
        ysT = qkT
        mergedT = guT
        xn2 = S.carve("E", "xn2", 2048, BF16)
        h2T = S.carve("E", "h2T", 2048, BF16, "p (k n) -> p k n", k=KT)
        qT = S.carve("E", "qT", 4096, BF16, "p (k n) -> p k n", k=16)
        W8 = S.carve("E", "W8", 8192, F32)
        v1 = S.carve("E", "v1", 1024, F32, "p (k n) -> p k n", k=16)
        i1 = S.carve("E", "i1", 1024, U32, "p (k n) -> p k n", k=16)
        i1f = S.carve("E", "i1f", 1024, F32, "p (k n) -> p k n", k=16)
        tsv = S.carve("E", "ts", 512, F32, "p (k n) -> p k n", k=8)
        tiv = S.carve("E", "ti", 512, U32, "p (k n) -> p k n", k=8)
        tab = S.carve("E", "tab", 1024, U32)
        abf = S.carve("E", "abf", 1024, F32)
        k12 = S.carve("E", "k12", 1024, F32)
        ef = S.carve("E", "ef", 512, F32)
        gex = S.carve("E", "gex", 512, F32, "p (k n) -> p k n", k=8)
        s3 = W8.a.rearrange("p (k n) -> p k n", k=16)
        s_hp = [S.sub(W8, s3[:, hp, :], "s%d" % hp) for hp in range(16)]
        c3 = W8.a.rearrange("p (k n) -> p k n", k=8)
        c_h = [S.sub(W8, c3[:, h, :], "c%d" % h) for h in range(8)]
        oh = S.sub(W8, W8.a.rearrange("p (h k j) -> p h k j", h=8, k=16), "oh")
        S.alias(s_hp, c_h)
        S.alias(s_hp, [oh])
        S.alias(c_h, [oh])
        v1_hp = [S.sub(v1, v1.a[:, hp, :]) for hp in range(16)]
        i1_hp = [S.sub(i1, i1.a[:, hp, :]) for hp in range(16)]
        ts_h = [S.sub(tsv, tsv.a[:, h, :]) for h in range(8)]
        ti_h = [S.sub(tiv, tiv.a[:, h, :]) for h in range(8)]
        pre_c = [S.sub(pre, pre.a[:, j:j + 1]) for j in range(128)]
        gl_c = [S.sub(glt, glt.a[:, j:j + 1]) for j in range(128)]
        glg_c = [S.sub(glg, glg.a[:, j:j + 1]) for j in range(128)]
        ss = Tk(sm.a[:, 0:1], "ss")
        rstd = Tk(sm.a[:, 1:2], "rstd")
        bst = Tk(sm.a[:, 2:26].rearrange("p (h s) -> p h s", h=4), "bst")
        bmv = Tk(sm.a[:, 26:34].rearrange("p (h s) -> p h s", h=4), "bmv")
        brs = Tk(sm.a[:, 34:38], "brs")
        lst = Tk(sm.a[:, 38:50], "lst")
        lmv = Tk(sm.a[:, 50:52], "lmv")
        lrs = Tk(sm.a[:, 52:53], "lrs")
        zsum = Tk(sm.a[:, 53:61], "zsum")
        ssb = Tk(sm.a[:, 61:62], "ssb")
        rstdb = Tk(sm.a[:, 62:63], "rstdb")

        def dma(eng, out, in_, key, reads=(), writes=()):
            S.op(eng, lambda e: e.dma_start(out=out, in_=in_), reads, writes, dma=key)

        def mm(out, lhsT, rhs, start, stop, reads, writes):
            S.op("pe", lambda e: e.matmul(out, lhsT=lhsT, rhs=rhs, start=start, stop=stop), reads, writes)

        def tr(out, in_, reads, writes):
            S.op("pe", lambda e: e.transpose(out=out, in_=in_, identity=identb.a), list(reads) + [identb], writes)

        def act(out, in_, func, reads, writes, **kw):
            S.op("act", lambda e: e.activation(out=out, in_=in_, func=func, **kw), reads, writes)

        def tt(eng, out, in0, in1, op, reads, writes):
            S.op(eng, lambda e: e.tensor_tensor(out=out, in0=in0, in1=in1, op=op), reads, writes)

        def tsc(eng, out, in0, s1, s2, op0, op1, reads, writes):
            if s2 is None:
                S.op(eng, lambda e: e.tensor_scalar(out=out, in0=in0, scalar1=s1, scalar2=None, op0=op0), reads, writes)
            else:
                S.op(eng, lambda e: e.tensor_scalar(out=out, in0=in0, scalar1=s1, scalar2=s2, op0=op0, op1=op1), reads, writes)

        def stt(eng, out, in0, scalar, in1, op0, op1, reads, writes, accum=None):
            if accum is None:
                S.op(eng, lambda e: e.scalar_tensor_tensor(out=out, in0=in0, scalar=scalar, in1=in1, op0=op0, op1=op1), reads, writes)
            else:
                S.op(eng, lambda e: e.scalar_tensor_tensor(out=out, in0=in0, scalar=scalar, in1=in1, op0=op0, op1=op1, accum_out=accum), reads, writes)

        def cp(eng, out, in_, reads, writes):
            if eng == "act":
                S.op(eng, lambda e: e.activation(out=out, in_=in_, func=AF.Copy), reads, writes)
            else:
                S.op(eng, lambda e: e.tensor_copy(out=out, in_=in_), reads, writes)

        def rstd_from(src_tk, src_ap, dst_tk, dst_ap, mul):
            tsc("dve", dst_ap, src_ap, mul, EPS, ALU.mult, ALU.add, [src_tk], [dst_tk])
            act(dst_ap, dst_ap, AF.Sqrt, [dst_tk], [dst_tk])
            S.op("dve", lambda e: e.reciprocal(out=dst_ap, in_=dst_ap), [dst_tk], [dst_tk])

        dma("sp", identb.a, identb_d, "c0", writes=[identb])
        dma("sp", cm.a, cmisc_d, "c1", writes=[cm])
        dma("sp", cols.a, cols_d, "c2", writes=[cols])
        dma("sp", ccol.a, ccol_d, "c3", writes=[ccol])
        dma("sp", modrow.a[0:1, :], bada_d, "c4", writes=[modrow])
        dma("sp", fgb.a, fg_d.partition_broadcast(128), "c5", writes=[fgb])
        S.op("dve", lambda e: e.memset(onesb.a, 1.0), writes=[onesb])
        S.op("dve", lambda e: e.memset(state.a, 0.0), writes=[state])
        S.op("dve", lambda e: e.memset(stateb.a, 0.0), writes=[stateb])
        winb_tk = [Tk(None, "winb%d" % j) for j in range(NPA)]
        win_v = win_d.rearrange("(kt p) n -> p kt n", p=128)
        wq_v = wq_d.rearrange("(kt p) n -> p kt n", p=128)
        for j in range(NPA):
            src = win_v[:, :, j * 512:(j + 1) * 512] if j < NPIECE else wq_v[:, :, (j - NPIECE) * 512:(j - NPIECE + 1) * 512]
            dma("pool", winb_d[j].rearrange("p (k n) -> p k n", k=KT), src, "wc", writes=[winb_tk[j]])
        for j in range(NPA):
            winb_tk[j].w = (S._dsem("wc"), 16 * NPA)
        ws_v = ws_d.rearrange("(kt p) n -> p kt n", p=128)
        for kt in range(KT):
            dma("pool", Ws.a[:, kt, :], ws_v[:, kt, :], "wsg", writes=[Ws])
        puv_tk = Tk(None, "puv")
        for r in range(16):
            dma("pool", puv_d[r * 1024:(r + 1) * 1024, 0:D], pu_d[r * 1024:(r + 1) * 1024, :], "uvc")
            dma("pool", puv_d[r * 1024:(r + 1) * 1024, D:2 * D], pv_d[r * 1024:(r + 1) * 1024, :], "uvc")
        puv_tk.w = (S._dsem("uvc"), 16 * 32)
        act(cact.a, ccol.a, AF.Silu, [ccol], [cact])
        wada_v = wada_d.rearrange("(kt p) n -> p kt n", p=128)
        for j in range(48):
            wb = wab[j % 2]
            dma("sp", wb.a, wada_v[:, :, j * 128:(j + 1) * 128], "wa%d" % (j % 2), writes=[wb])
            pb = nb()
            for kt in range(KT):
                mm(pb.a[0:1, 0:128], cact.a[:, kt:kt + 1], wb.a[:, kt, :], kt == 0, kt == KT - 1, [cact, wb], [pb])
            tt("dve", modrow.a[0:1, j * 128:(j + 1) * 128], pb.a[0:1, 0:128], modrow.a[0:1, j * 128:(j + 1) * 128],
               ALU.add, [pb, modrow], [modrow])
        pb = nb()
        for si, sec in enumerate((0, 1, 3, 4)):
            for kt in range(KT):
                c0 = sec * D + kt * 128
                mm(pb.a[:, si * 8 + kt:si * 8 + kt + 1], modrow.a[0:1, c0:c0 + 128], ones_f[0:1, 0:1], True, True,
                   [modrow, cm], [pb])
        cp("dve", modcol.a, pb.a[:, 0:32], [pb], [modcol])
        stt("dve", a1col, sc1col, 1.0, g1col, ALU.add, ALU.mult, [modcol, cols], [acol])
        stt("dve", a2col, sc2col, 1.0, g2col, ALU.add, ALU.mult, [modcol, cols], [acol])
        for half in range(2):
            pb = nb()
            mm(pb.a, ones_f[0:1, :], modrow.a[0:1, 2 * D + half * 512:2 * D + (half + 1) * 512], True, True, [modrow, cm], [pb])
            cp("act", gate1b.a[:, half * 512:(half + 1) * 512], pb.a, [pb], [gate1b])
            pb = nb()
            mm(pb.a, ones_f[0:1, :], modrow.a[0:1, 5 * D + half * 512:5 * D + (half + 1) * 512], True, True, [modrow, cm], [pb])
            cp("act", gate2b.a[:, half * 512:(half + 1) * 512], pb.a, [pb], [gate2b])
        wr_v = wr_d.rearrange("(kt p) n -> p kt n", p=128)
        wo_v = wo_d.rearrange("(kt p) n -> p kt n", p=128)
        for kt in range(KT):
            st_ = stage[0]
            dma("sp", st_.a, wr_v[:, kt, :], "st0", writes=[st_])
            act(Wr.a[:, kt, :], st_.a, AF.Copy, [st_, cols], [Wr], scale=gncol[:, kt:kt + 1])
            st_ = stage[1]
            dma("sp", st_.a, wo_v[:, kt, :], "st0", writes=[st_])
            tt("dve", Wo.a[:, kt, :], st_.a, gate1b.a, ALU.mult, [st_, gate1b], [Wo])
        dma("sp", kst.a, keys_d.rearrange("k n c -> n k c"), "ks", writes=[kst])
        cp("act", kstb.a, kst.a, [kst], [kstb])
        for g in range(2):
            pb = nb()
            pbb = pb.a.bitcast(BF16).rearrange("p (k n) -> p k n", k=8)
            for j in range(8):
                tr(pbb[:, j, :], kstb.a[:, g * 8 + j, :], [kstb], [pb])
            cp("dve", keysT.a[:, g * 8:(g + 1) * 8, :], pbb, [pb], [keysT])
        dma("sp", wst.a, sguw_d.rearrange("g t s -> t g s"), "wt", writes=[wst])
        dma("sp", bsrow.a[0:1, 0:512], sgub_d, "bs", writes=[bsrow])
        cp("act", wstb.a, wst.a, [wst], [wstb])
        pb = nb()
        pbb = pb.a.bitcast(BF16).rearrange("p (k n) -> p k n", k=8)
        for g in range(4):
            tr(pbb[:, g, :], wstb.a[:, g, :], [wstb], [pb])
        tt("dve", wsT.a, pbb[:, 0:4, :], trilT.unsqueeze(1).broadcast_to([128, 4, 128]), ALU.mult, [pb, cm], [wsT])
        pA = nb()
        pB = nb()
        pA3 = pA.a.rearrange("p (k n) -> p k n", k=4)
        pB3 = pB.a.rearrange("p (k n) -> p k n", k=4)
        for g in range(4):
            mm(pA3[:, g, :], onesb.a, wsT.a[:, g, :], True, True, [onesb, wsT], [pA])
            mm(pB3[:, g, :], ones_f[0:1, :], bsrow.a[0:1, g * 128:(g + 1) * 128], True, True, [cm, bsrow], [pB])
        cp("act", bsBs.a, pB3, [pB], [bsBs])
        for ct in range(8):
            stt("dve", bsBp.a[:, ct, :], pA3[:, ct // 2, :], lnbcol[:, ct:ct + 1], bsBs.a[:, ct // 2, :], ALU.mult, ALU.add,
                [pA, cols, bsBs], [bsBp])

        total_pieces = NCH * NPA
        issued = [0]

        def ensure_w(upto):
            while issued[0] <= min(upto, total_pieces - 1):
                g_ = issued[0]
                j_ = g_ % NPA
                wb_ = wbuf[g_ % 2]
                dma("sp", wb_.a, winb_d[j_].rearrange("p (k n) -> p k n", k=KT), "wb%d" % (g_ % 2),
                    reads=[winb_tk[j_]], writes=[wb_])
                issued[0] += 1

        dma("sp", xts[0].a, x_d[0:128, :], "x0", writes=[xts[0]])
        dma("sp", cs.a, cs_d[0], "cs", writes=[cs])
        ensure_w(0)

        def norm_transpose(xt, xn_t, hT_t, acolv, shcolv):
            S.op("dve", lambda e: e.memset(ss.a, 0.0), writes=[ss])
            act(junkA.a, xt.a, AF.Square, [xt, ss], [ss, junkA], accum_out=ss.a)
            rstd_from(ss, ss.a, rstd, rstd.a, 1.0 / D)
            act(xn_t.a, xt.a, AF.Copy, [xt, rstd], [xn_t], scale=rstd.a)
            pb = nb()
            pbb = pb.a.bitcast(BF16).rearrange("p (k n) -> p k n", k=8)
            for kt in range(KT):
                tr(pbb[:, kt, :], xn_t.a[:, kt * 128:(kt + 1) * 128], [xn_t], [pb])
            for kt in range(KT):
                tsc("dve", hT_t.a[:, kt, :], pbb[:, kt, :], acolv[:, kt:kt + 1], shcolv[:, kt:kt + 1], ALU.mult, ALU.add,
                    [pb, acol, modcol], [hT_t])

        def front(i):
            xt = xts[i % 3]
            h2tok, eidx, gg = h2toks[i % 2], eidxs[i % 2], ggs[i % 2]
            norm_transpose(xt, xn, hT, a1col, sh1col)
            yield
            for j in range(NPIECE):
                gidx = i * NPA + j
                ensure_w(gidx + 1)
                wb = wbuf[gidx % 2]
                pb = nb()
                fm = j in (6, 7, 10, 11, 12, 13)
                if not fm:
                    for kt in range(KT):
                        mm(pb.a, hT.a[:, kt, :], wb.a[:, kt, :], kt == 0, kt == KT - 1, [hT, wb], [pb])
                else:
                    p3 = pb.a.rearrange("p (k n) -> p k n", k=4)
                    for n in range(4):
                        for kt in range(KT):
                            mm(p3[:, n, :], wb.a[:, kt, n * 128:(n + 1) * 128], hT.a[:, kt, :], kt == 0, kt == KT - 1,
                               [hT, wb], [pb])
                if j < 2:
                    act(qk.a[:, j * 4:(j + 1) * 4, :], pb.a.rearrange("p (h n) -> p h n", h=4), AF.Copy, [pb], [qk])
                elif j < 4:
                    act(vv.a[:, (j - 2) * 512:(j - 1) * 512], pb.a, AF.Copy, [pb], [vv])
                elif j < 6:
                    act(sg.a[:, (j - 4) * 512:(j - 3) * 512], pb.a, AF.Silu, [pb], [sg])
                elif j < 8:
                    act(guT.a[:, (j - 6) * 4:(j - 5) * 4, :], p3, AF.Gelu, [pb], [guT])
                elif j < 10:
                    act(gsv.a[:, (j - 8) * 512:(j - 7) * 512], pb.a, AF.Gelu, [pb], [gsv])
                elif j < 12:
                    act(sigA.a[:, (j - 10) * 4:(j - 9) * 4, :], p3, AF.Sigmoid, [pb], [sigA])
                else:
                    act(sigB.a[:, (j - 12) * 4:(j - 11) * 4, :], p3, AF.Sigmoid, [pb], [sigB])
                if j == 1:
                    c2 = cs.a[:, 0:128].unsqueeze(1).broadcast_to([128, 8, 128])
                    sn = cs.a[:, 128:192].unsqueeze(1).broadcast_to([128, 8, 64])
                    sp_ = cs.a[:, 192:256].unsqueeze(1).broadcast_to([128, 8, 64])
                    tt("dve", rA.a, qk.a, c2, ALU.mult, [qk, cs], [rA])
                    tt("dve", rB.a[:, :, 0:64], qk.a[:, :, 64:128], sn, ALU.mult, [qk, cs], [rB])
                    tt("dve", rB.a[:, :, 64:128], qk.a[:, :, 0:64], sp_, ALU.mult, [qk, cs], [rB])
                    tt("dve", rA.a, rA.a, rB.a, ALU.add, [rA, rB], [rA])
                    yield
                    for h in range(4):
                        act(qd.a[:, h, :], rA.a[:, h, :], AF.Copy, [rA, cm], [qd], scale=dq[:, h:h + 1])
                        act(kp.a[:, h, :], rA.a[:, 4 + h, :], AF.Copy, [rA, cm], [kp], scale=dk[:, h:h + 1])
                    pt_ = nb()
                    ptb = pt_.a.bitcast(BF16).rearrange("p (k n) -> p k n", k=8)
                    for h in range(4):
                        tr(ptb[:, h, :], qd.a[:, h, :], [qd], [pt_])
                        tr(ptb[:, 4 + h, :], kp.a[:, h, :], [kp], [pt_])
                    cp("act", qkT.a, ptb, [pt_], [qkT])
                    psc = nb()
                    ps3 = psc.a.rearrange("p (h n) -> p h n", h=4)
                    for h in range(4):
                        mm(ps3[:, h, :], qkT.a[:, 4 + h, :], qkT.a[:, h, :], True, True, [qkT], [psc])
                    tt("dve", sTm.a, ps3, trilT.unsqueeze(1).broadcast_to([128, 4, 128]), ALU.mult, [psc, cm], [sTm])
                yield
            pY = nb(2)
            pD = nb(2)
            for h in range(4):
                o = pY[h // 2].a[:, (h % 2) * 256:(h % 2 + 1) * 256]
                mm(o, sTm.a[:, h, :], vv.a[:, h * 256:(h + 1) * 256], True, False, [sTm, vv], [pY[h // 2]])
                mm(o, qkT.a[:, h, :], stateb.a[:, h * 256:(h + 1) * 256], False, True, [qkT, stateb], [pY[h // 2]])
            for h in range(4):
                o = pD[h // 2].a[:, (h % 2) * 256:(h % 2 + 1) * 256]
                mm(o, kp.a[:, h, :], vv.a[:, h * 256:(h + 1) * 256], True, True, [kp, vv], [pD[h // 2]])
            tmpS_a = qk.a.rearrange("p h n -> p (h n)")
            for hh in range(2):
                tt("dve", tmpS_a[:, hh * 512:(hh + 1) * 512], pD[hh].a, state.a[:, hh * 512:(hh + 1) * 512], ALU.add,
                   [pD[hh], state], [qk])
            for h in range(4):
                act(state.a[:, h * 256:(h + 1) * 256], tmpS_a[:, h * 256:(h + 1) * 256], AF.Copy, [qk], [state],
                    scale=float(gC[h]))
            cp("act", stateb.a, state.a, [state], [stateb])
            for h in range(4):
                src = pY[h // 2].a[:, (h % 2) * 256:(h % 2 + 1) * 256]
                S.op("dve", lambda e, src=src, h=h: e.bn_stats(out=bst.a[:, h, :], in_=src), [pY[h // 2]], [bst])
            for h in range(4):
                S.op("dve", lambda e, h=h: e.bn_aggr(out=bmv.a[:, h, :], in_=bst.a[:, h, :]), [bst], [bmv])
            rstd_from(bmv, bmv.a[:, :, 1], brs, brs.a, 1.0)
            yn = rA.a.rearrange("p h n -> p (h n)")
            for h in range(4):
                src = pY[h // 2].a[:, (h % 2) * 256:(h % 2 + 1) * 256]
                tsc("dve", yn[:, h * 256:(h + 1) * 256], src, bmv.a[:, h, 0:1], brs.a[:, h:h + 1], ALU.subtract, ALU.mult,
                    [pY[h // 2], bmv, brs], [rA])
            tt("dve", zz.a, sg.a, yn, ALU.mult, [sg, rA], [zz])
            yield
            pt_ = nb()
            ptb = pt_.a.bitcast(BF16).rearrange("p (k n) -> p k n", k=8)
            for kt in range(KT):
                tr(ptb[:, kt, :], zz.a[:, kt * 128:(kt + 1) * 128], [zz], [pt_])
            cp("act", zT.a, ptb, [pt_], [zT])
            pA_ = nb(2)
            for n in range(8):
                o = pA_[n // 4].a.rearrange("p (k n) -> p k n", k=4)[:, n % 4, :]
                for kt in range(KT):
                    mm(o, Wr.a[:, kt, n * 128:(n + 1) * 128], zT.a[:, kt, :], kt == 0, kt == KT - 1, [Wr, zT], [pA_[n // 4]])
                if n == 3:
                    yield
            m1 = gsv.a.rearrange("p (k n) -> p k n", k=KT)
            m2 = sg.a.rearrange("p (k n) -> p k n", k=KT)
            for c in range(2):
                S.op("dve", lambda e, c=c: e.bn_stats(out=lst.a[:, c * 6:(c + 1) * 6], in_=gsv.a[:, c * 512:(c + 1) * 512]),
                     [gsv], [lst])
            S.op("dve", lambda e: e.bn_aggr(out=lmv.a, in_=lst.a), [lst], [lmv])
            rstd_from(lmv, lmv.a[:, 1:2], lrs, lrs.a, 1.0)
            tsc("dve", vhat.a, gsv.a, lmv.a[:, 0:1], lrs.a, ALU.subtract, ALU.mult, [gsv, lmv, lrs], [vhat])
            yield
            for hh in range(2):
                tt("dve", m1[:, hh * 4:(hh + 1) * 4, :], pA_[hh].a.rearrange("p (k n) -> p k n", k=4),
                   sigA.a[:, hh * 4:(hh + 1) * 4, :], ALU.mult, [pA_[hh], sigA], [gsv])
            pM = nb(2)
            for ct in range(8):
                o = pM[ct // 4].a.rearrange("p (k n) -> p k n", k=4)[:, ct % 4, :]
                mm(o, vhat.a[:, ct * 128:(ct + 1) * 128], wsT.a[:, ct // 2, :], True, True, [vhat, wsT], [pM[ct // 4]])
            t1 = rB.a
            for ct in range(8):
                src = pM[ct // 4].a.rearrange("p (k n) -> p k n", k=4)[:, ct % 4, :]
                stt("dve", t1[:, ct, :], src, lngcol[:, ct:ct + 1], bsBp.a[:, ct, :], ALU.mult, ALU.add,
                    [pM[ct // 4], cols, bsBp], [rB])
            tt("dve", ysT.a, guT.a, t1, ALU.mult, [guT, rB], [ysT])
            yield
            pB_ = nb(2)
            for n in range(8):
                o = pB_[n // 4].a.rearrange("p (k n) -> p k n", k=4)[:, n % 4, :]
                for kt in range(KT):
                    mm(o, Ws.a[:, kt, n * 128:(n + 1) * 128], ysT.a[:, kt, :], kt == 0, kt == KT - 1, [Ws, ysT], [pB_[n // 4]])
                if n == 3:
                    yield
            for hh in range(2):
                tt("dve", m2[:, hh * 4:(hh + 1) * 4, :], pB_[hh].a.rearrange("p (k n) -> p k n", k=4),
                   sigB.a[:, hh * 4:(hh + 1) * 4, :], ALU.mult, [pB_[hh], sigB], [sg])
            tt("dve", mergedT.a, m1, m2, ALU.add, [gsv, sg], [mergedT])
            yield
            pO = nb(2)
            for hh in range(2):
                for kt in range(KT):
                    mm(pO[hh].a, mergedT.a[:, kt, :], Wo.a[:, kt, hh * 512:(hh + 1) * 512], kt == 0, kt == KT - 1,
                       [mergedT, Wo], [pO[hh]])
            for hh in range(2):
                tt("dve", xt.a[:, hh * 512:(hh + 1) * 512], pO[hh].a, xt.a[:, hh * 512:(hh + 1) * 512], ALU.add,
                   [pO[hh], xt], [xt])
            yield
            norm_transpose(xt, xn2, h2T, a2col, sh2col)
            pt_ = nb()
            ptb = pt_.a.bitcast(BF16)
            for kt in range(KT):
                tr(ptb[:, kt * 128:(kt + 1) * 128], h2T.a[:, kt, :], [h2T], [pt_])
            cp("act", h2tok.a, ptb, [pt_], [h2tok])
            yield
            for p_ in range(4):
                gidx = i * NPA + NPIECE + p_
                ensure_w(gidx + 1)
                wb = wbuf[gidx % 2]
                pb = nb()
                p3 = pb.a.rearrange("p (k n) -> p k n", k=4)
                for n in range(4):
                    for kt in range(KT):
                        mm(p3[:, n, :], wb.a[:, kt, n * 128:(n + 1) * 128], h2T.a[:, kt, :], kt == 0, kt == KT - 1,
                           [h2T, wb], [pb])
                cp("act", qT.a[:, p_ * 4:(p_ + 1) * 4, :], p3, [pb], [qT])
                yield
            for g in range(4):
                pb = nb()
                p3 = pb.a.rearrange("p (k n) -> p k n", k=4)
                for n in range(4):
                    hp = g * 4 + n
                    mm(p3[:, n, :], qT.a[:, hp, :], keysT.a[:, hp, :], True, True, [qT, keysT], [pb])
                cp("act", s3[:, g * 4:(g + 1) * 4, :], p3, [pb], s_hp[g * 4:(g + 1) * 4])
            yield
            for rnd in range(2):
                lo, hi = rnd * 8, rnd * 8 + 8
                for hp in range(16):
                    S.op("dve", lambda e, hp=hp, lo=lo, hi=hi: e.max(out=v1.a[:, hp, lo:hi], in_=s3[:, hp, :]),
                         [s_hp[hp]], [v1_hp[hp]])
                    if hp % 8 == 7:
                        yield
                for hp in range(16):
                    S.op("dve", lambda e, hp=hp, lo=lo, hi=hi: e.max_index(out=i1.a[:, hp, lo:hi], in_max=v1.a[:, hp, lo:hi],
                                                                          in_values=s3[:, hp, :]),
                         [s_hp[hp], v1_hp[hp]], [i1_hp[hp]])
                    if hp % 8 == 7:
                        yield
                if rnd == 0:
                    for hp in range(16):
                        S.op("dve", lambda e, hp=hp: e.match_replace(out=s3[:, hp, :], in_to_replace=v1.a[:, hp, 0:8],
                                                                     in_values=s3[:, hp, :], imm_value=-1e30),
                             [v1_hp[hp]], [s_hp[hp]])
                        if hp % 8 == 7:
                            yield
            v14 = v1.a.rearrange("p (h two) k -> p h two k", two=2)
            c4 = W8.a.rearrange("p (h a b) -> p h a b", h=8, a=16)
            tt("dve", c4, v14[:, :, 0, :].unsqueeze(3).broadcast_to([128, 8, 16, 16]),
               v14[:, :, 1, :].unsqueeze(2).broadcast_to([128, 8, 16, 16]), ALU.add, v1_hp, c_h)
            yield
            for rnd in range(2):
                lo, hi = rnd * 8, rnd * 8 + 8
                for h in range(8):
                    S.op("dve", lambda e, h=h, lo=lo, hi=hi: e.max(out=tsv.a[:, h, lo:hi], in_=c3[:, h, :]), [c_h[h]], [ts_h[h]])
                for h in range(8):
                    S.op("dve", lambda e, h=h, lo=lo, hi=hi: e.max_index(out=tiv.a[:, h, lo:hi], in_max=tsv.a[:, h, lo:hi],
                                                                        in_values=c3[:, h, :]),
                         [c_h[h], ts_h[h]], [ti_h[h]])
                yield
                if rnd == 0:
                    for h in range(8):
                        S.op("dve", lambda e, h=h: e.match_replace(out=c3[:, h, :], in_to_replace=tsv.a[:, h, 0:8],
                                                                   in_values=c3[:, h, :], imm_value=-1e30),
                             [ts_h[h]], [c_h[h]])
                yield
            ti2 = tiv.a.rearrange("p h k -> p (h k)")
            S.op("dve", lambda e: e.tensor_single_scalar(out=tab.a[:, 0:128], in_=ti2, scalar=4, op=ALU.logical_shift_right),
                 ti_h, [tab])
            S.op("dve", lambda e: e.tensor_single_scalar(out=tab.a[:, 128:256], in_=ti2, scalar=15, op=ALU.bitwise_and),
                 ti_h, [tab])
            cp("dve", abf.a, tab.a, [tab], [abf])
            cp("dve", i1f.a, i1.a, i1_hp, [i1f])
            i1f4 = i1f.a.rearrange("p (h two) k -> p h two k", two=2)
            io4 = iota16.unsqueeze(1).unsqueeze(1).broadcast_to([128, 8, 16, 16])
            for p_ in range(2):
                sel = abf.a[:, p_ * 128:(p_ + 1) * 128].rearrange("p (h k) -> p h k", h=8)
                tt("dve", oh.a, sel.unsqueeze(3).broadcast_to([128, 8, 16, 16]), io4, ALU.is_equal, [abf, cm], [oh])
                yield
                tt("dve", oh.a, oh.a, i1f4[:, :, p_, :].unsqueeze(2).broadcast_to([128, 8, 16, 16]), ALU.mult, [oh, i1f], [oh])
                yield
                S.op("dve", lambda e, p_=p_: e.tensor_reduce(out=k12.a[:, p_ * 128:(p_ + 1) * 128],
                                                            in_=oh.a.rearrange("p h k j -> p (h k) j"), axis=AX.X, op=ALU.add),
                     [oh], [k12])
                yield
            stt("dve", ef.a, k12.a[:, 0:128], 128.0, k12.a[:, 128:256], ALU.mult, ALU.add, [k12], [ef])
            tsc("dve", ef.a, ef.a, 0.0, 16383.0, ALU.max, ALU.min, [ef], [ef])
            cp("dve", eidx.a, ef.a, [ef], [eidx])
            tt("dve", gex.a, tsv.a, tsv.a[:, :, 0:1].broadcast_to([128, 8, 16]), ALU.subtract, ts_h, [gex])
            act(gex.a, gex.a, AF.Exp, [gex], [gex])
            S.op("dve", lambda e: e.tensor_reduce(out=zsum.a, in_=gex.a, axis=AX.X, op=ALU.add), [gex], [zsum])
            S.op("dve", lambda e: e.reciprocal(out=zsum.a, in_=zsum.a), [zsum], [zsum])
            tt("dve", gg.a, gex.a, zsum.a.unsqueeze(2).broadcast_to([128, 8, 16]), ALU.mult, [gex, zsum], [gg])
            yield

        gi = [0]
        def back(i):
            pP = banks[4:6] if i % 2 == 0 else banks[6:8]
            xt = xts[i % 3]
            h2tok, eidx, gg = h2toks[i % 2], eidxs[i % 2], ggs[i % 2]
            ggf = gg.a.rearrange("p h k -> p (h k)")
            S.op("dve", lambda e: e.memset(pre.a, 0.0), writes=pre_c)
            G = 2
            NG = 128 // G
            hk_slot = {}

            def stageA(g):
                for hk in range(g * G, (g + 1) * G):
                    sl = slots[gi[0] % NSLOT]
                    key = "g%d" % (gi[0] % NSLOT)
                    jd = junkDs[gi[0] % 2]
                    gi[0] += 1
                    hk_slot[hk] = sl
                    S.op("pool", lambda e, sl=sl, hk=hk: e.indirect_dma_start(
                        out=sl.a, out_offset=None, in_=puv_d,
                        in_offset=bass.IndirectOffsetOnAxis(ap=eidx.a[:, hk:hk + 1], axis=0)),
                        [eidx, puv_tk], [sl], dma=key)
                    stt("dve", jd.a, sl.a[:, 0:D], 1.0, h2tok.a, ALU.mult, ALU.mult, [sl, h2tok, pre_c[hk]],
                        [pre_c[hk], jd], accum=pre.a[:, hk:hk + 1])

            def stageC(g):
                c0, c1 = g * G, (g + 1) * G
                act(glt.a[:, c0:c1], pre.a[:, c0:c1], AF.Gelu, pre_c[c0:c1], gl_c[c0:c1])
                for hk in range(c0, c1):
                    act(glg.a[:, hk:hk + 1], glt.a[:, hk:hk + 1], AF.Copy, [gl_c[hk], gg], [glg_c[hk]], scale=ggf[:, hk:hk + 1])
                for hk in range(c0, c1):
                    sl = hk_slot.pop(hk)
                    dgt = dg[hk % 4]
                    act(dgt.a, identf, AF.Copy, [cm, glg_c[hk]], [dgt], scale=glg.a[:, hk:hk + 1])
                    for hh in range(2):
                        mm(pP[hh].a, dgt.a, sl.a[:, D + hh * 512:D + (hh + 1) * 512], hk == 0, hk == 127, [dgt, sl], [pP[hh]])

            for step in range(NG + 1):
                if step < NG:
                    stageA(step)
                if step >= 1:
                    stageC(step - 1)
                yield
            yield "T"
            for hh in range(2):
                tt("dve", rtmp.a, pP[hh].a, gate2b.a[:, hh * 512:(hh + 1) * 512], ALU.mult, [pP[hh], gate2b], [rtmp])
                tt("dve", xt.a[:, hh * 512:(hh + 1) * 512], rtmp.a, xt.a[:, hh * 512:(hh + 1) * 512], ALU.add, [rtmp, xt], [xt])
            S.op("dve", lambda e: e.memset(ssb.a, 0.0), writes=[ssb])
            act(junkA.a, xt.a, AF.Square, [xt, ssb], [ssb, junkA], accum_out=ssb.a)
            rstd_from(ssb, ssb.a, rstdb, rstdb.a, 1.0 / D)
            stt("dve", xt.a, xt.a, rstdb.a, fgb.a, ALU.mult, ALU.mult, [xt, rstdb, fgb], [xt])
            S.op("act", lambda e, i=i, xt=xt: e.dma_start(out=out_d[i * 128:(i + 1) * 128, :], in_=xt.a), [xt], [], dma="out%d" % (i % 3))
            yield

        def prefetch_x(i):
            if i < NCH:
                dma("sp", xts[i % 3].a, x_d[i * 128:(i + 1) * 128, :], "x%d" % (i % 3), writes=[xts[i % 3]])

        def drain(g):
            for _ in g:
                pass

        drain(front(0))
        FRAC = 0.9
        RUNMAX = 4
        DVEW = 2.5
        KDEF = 6
        nbk = 128 // 2 + 1
        pending = None
        for i in range(NCH):
            b = back(i)
            bd = 0
            for _ in range(KDEF):
                next(b)
                bd += 1
            if pending is not None:
                drain(pending)
            bdone = False

            def step_back():
                nonlocal_state = None
                return next(b)

            if i + 1 < NCH:
                prefetch_x(i + 1)
                dma("sp", cs.a, cs_d[i + 1], "cs", writes=[cs])
                S.rec = []
                drain(front(i + 1))
                ops, S.rec = S.rec, None
                n_ops = len(ops)
                wts = [DVEW if o[0] == "dve" else 1.0 for o in ops]
                wtot = sum(wts)
                wacc = 0.0
                run = 0
                for k, (eng, fn, rd, wr, dm) in enumerate(ops):
                    S.op(eng, fn, rd, wr, dm)
                    run += 1
                    wacc += wts[k]
                    if k + 1 < n_ops and ops[k + 1][0] == eng and run < RUNMAX:
                        continue
                    run = 0
                    while not bdone and bd < KDEF + (wacc / wtot) * FRAC * (nbk - KDEF):
                        if next(b) == "T":
                            bdone = True
                        else:
                            bd += 1
            while not bdone:
                if next(b) == "T":
                    bdone = True
            pending = b
        drain(pending)

        fin = []
        for kk in ("out0", "out1", "out2"):
            if kk in S.dsem:
                fin.append((S.dsem[kk], S.cnt[S.dsem[kk]]))
        S.finish(fin)
        S.run()
    return nc, S.nins


def _consts(NCH):
    H, C = 4, 128
    gamma = 1.0 - 2.0 ** (-5.0 - np.arange(H, dtype=np.float64))
    i = np.arange(C, dtype=np.float64)
    cm = np.zeros((128, 408), np.float32)
    cm[:, 0:128] = (np.arange(128)[None, :] >= np.arange(128)[:, None]).astype(np.float32)
    cm[:, 128:132] = (gamma[None, :] ** (i[:, None] + 1.0)).astype(np.float32)
    cm[:, 132:136] = (gamma[None, :] ** (-(i[:, None] + 1.0)) * (128.0 ** -0.5)).astype(np.float32)
    cm[:, 136:152] = np.arange(16, dtype=np.float32)[None, :]
    cm[:, 152:280] = 1.0
    cm[:, 280:408] = np.eye(128, dtype=np.float32)
    gC = (gamma ** C).astype(np.float64)
    half = 64
    inv = (np.float32(10000.0) ** (-np.arange(half, dtype=np.float32) * np.float32(2.0) / np.float32(128))).astype(np.float32)
    pos = np.arange(NCH * 128, dtype=np.float32)
    ang = (pos[:, None] * inv[None, :]).astype(np.float32)
    cos, sin = np.cos(ang).astype(np.float32), np.sin(ang).astype(np.float32)
    cs = np.concatenate([cos, cos, -sin, sin], axis=1).reshape(NCH, 128, 256).astype(np.float32)
    identb = np.eye(128, dtype=np.float32).astype(ml_dtypes.bfloat16)
    return cm, gC, cs, identb


def _col(v):
    return np.ascontiguousarray(np.asarray(v, np.float32).reshape(8, 128).T)


_CACHE = {}


def run_cores(inputs, NCH, cores):
    f = lambda k: np.asarray(inputs[k], np.float32)
    cm, gC, cs, identb = _consts(NCH)
    key = NCH
    if key not in _CACHE:
        _CACHE[key] = build_program(NCH, gC)
    nc, nins = _CACHE[key]
    cols = np.concatenate([_col(f("norm1_g")[0]), _col(f("norm2_g")[0]), _col(f("ret_gn_g")[0]),
                           _col(f("sgu_ln_g")[0]), _col(f("sgu_ln_b")[0])], axis=1)
    shared = {
        "w_ada": np.ascontiguousarray(f("w_ada")[0]), "b_ada": np.ascontiguousarray(f("b_ada")[0].reshape(1, -1)),
        "cols": np.ascontiguousarray(cols), "w_in": np.ascontiguousarray(f("w_in")[0]),
        "w_ret_out": np.ascontiguousarray(f("w_ret_out")[0]), "w_sgu_out": np.ascontiguousarray(f("w_sgu_out")[0]),
        "w_out": np.ascontiguousarray(f("w_out")[0]), "w_q": np.ascontiguousarray(f("peer_w_q")[0]),
        "keys": np.ascontiguousarray(f("peer_sub_keys")[0].reshape(16, 128, 128)),
        "sgu_w": np.ascontiguousarray(f("sgu_w")[0]), "sgu_b": np.ascontiguousarray(f("sgu_b")[0].reshape(1, 512)),
        "final_g": np.ascontiguousarray(f("final_g").reshape(1, -1)),
        "peer_u": np.ascontiguousarray(f("peer_u")[0]), "peer_v": np.ascontiguousarray(f("peer_v")[0]),
        "identb": identb, "cmisc": cm, "cs": cs,
    }
    x, c = f("x"), f("c")
    in_maps = []
    for b in cores:
        m = dict(shared)
        m["x"] = np.ascontiguousarray(x[b, :NCH * 128])
        m["ccol"] = _col(c[b])
        in_maps.append(m)
    res = run_bass_kernel_spmd(nc, in_maps, core_ids=list(range(len(cores))))
    return np.stack([np.asarray(r["out"], np.float32) for r in res.results], axis=0)


def kernel(**inputs):
    return run_cores(inputs, 32, list(range(8))).astype(np.float32)
```

```python
import numpy as np
from contextlib import ExitStack
import ml_dtypes
import concourse.bass as bass
import concourse.mybir as mybir
from concourse.bass_utils import run_bass_kernel_spmd

F32 = mybir.dt.float32
BF16 = mybir.dt.bfloat16
I32 = mybir.dt.int32
U32 = mybir.dt.uint32
AF = mybir.ActivationFunctionType
ALU = mybir.AluOpType
AX = mybir.AxisListType

D = 1024
KT = 8
NPIECE = 14
NPA = 18
EPS = 1e-6
NSLOT = 12
ARENA = 41 * 1024
INTERLEAVE = True


class Tk:
    __slots__ = ("a", "w", "r", "al", "name", "lay", "off", "size")

    def __init__(self, a, name=""):
        self.a = a
        self.w = None
        self.r = {}
        self.al = []
        self.name = name
        self.lay = None
        self.off = 0
        self.size = 0


class Sched:
    ENG = ("pe", "act", "dve", "pool", "sp")

    def __init__(self, nc, es):
        self.nc = nc
        self.es = es
        self.q = {e: [] for e in self.ENG}
        self.sem = {e: es.enter_context(nc.semaphore("s_" + e)) for e in self.ENG}
        self.cnt = {}
        self.known = {e: {} for e in self.ENG}
        self.dsem = {}
        self.arena = None
        self.lay_off = {}
        self.carved = []
        self.nins = {e: 0 for e in self.ENG}
        self.rec = None

    def tile(self, name, shape, dt):
        t = self.es.enter_context(self.nc.sbuf_tensor(name, list(shape), dt))
        return Tk(t[:], name)

    def psum(self, name, shape, dt):
        t = self.es.enter_context(self.nc.psum_tensor(name, list(shape), dt))
        return Tk(t[:], name)

    def carve(self, lay, name, nbytes, dt, pattern=None, **kw):
        if self.arena is None:
            self.arena = self.es.enter_context(self.nc.sbuf_tensor("arena", [128, ARENA // 4], F32))
        off = self.lay_off.get(lay, 0)
        nbytes = (nbytes + 31) // 32 * 32
        assert off + nbytes <= ARENA, (lay, name, off, nbytes)
        self.lay_off[lay] = off + nbytes
        a = self.arena[:, off // 4:(off + nbytes) // 4]
        if dt != F32:
            a = a.bitcast(dt)
        if pattern is not None:
            a = a.rearrange(pattern, **kw)
        t = Tk(a, name)
        t.lay, t.off, t.size = lay, off, nbytes
        for u in self.carved:
            if u.lay != lay and u.off < off + nbytes and off < u.off + u.size:
                u.al.append(t)
                t.al.append(u)
        self.carved.append(t)
        return t

    def sub(self, parent, a, name=""):
        t = Tk(a, name or parent.name)
        t.al = list(parent.al)
        for u in parent.al:
            u.al.append(t)
        return t

    @staticmethod
    def alias(xs, ys):
        for x in xs:
            for y in ys:
                x.al.append(y)
                y.al.append(x)

    def _dsem(self, key):
        if key not in self.dsem:
            self.dsem[key] = self.es.enter_context(self.nc.semaphore("d_" + key))
        return self.dsem[key]

    def op(self, eng, fn, reads=(), writes=(), dma=None):
        if self.rec is not None:
            self.rec.append((eng, fn, list(reads), list(writes), dma))
            return None
        deps = {}

        def add(d):
            if d is not None and deps.get(d[0], 0) < d[1]:
                deps[d[0]] = d[1]

        for t in reads:
            add(t.w)
            for u in t.al:
                add(u.w)
        for t in writes:
            if not t.r:
                add(t.w)
            for s, v in t.r.items():
                add((s, v))
            for u in t.al:
                if not u.r:
                    add(u.w)
                for s, v in u.r.items():
                    add((s, v))
        own = self.sem[eng]
        waits = []
        kn = self.known[eng]
        for s, v in deps.items():
            if eng == "pe" and s is own:
                continue
            if kn.get(s, 0) >= v:
                continue
            kn[s] = v
            waits.append((s, v))
        if dma is None:
            s, inc = own, 1
        else:
            s, inc = self._dsem(dma), 16
        val = self.cnt.get(s, 0) + inc
        self.cnt[s] = val
        self.nins[eng] += 1 + len(waits)

        def emit(e, waits=waits, fn=fn, s=s, inc=inc):
            for ws, wv in waits:
                e.wait_ge(ws, wv)
            fn(e).then_inc(s, inc)

        self.q[eng].append(emit)
        for t in writes:
            t.w = (s, val)
            t.r = {}
        for t in reads:
            if t.r.get(s, 0) < val:
                t.r[s] = val
        return (s, val)

    def finish(self, final_deps):
        def emit(e):
            for s, v in final_deps:
                e.wait_ge(s, v)
        self.q["sp"].append(emit)

    def run(self):
        with self.nc.Block() as block:
            @block.tensor
            def _(e):
                for f in self.q["pe"]:
                    f(e)

            @block.scalar
            def _(e):
                for f in self.q["act"]:
                    f(e)

            @block.vector
            def _(e):
                for f in self.q["dve"]:
                    f(e)

            @block.gpsimd
            def _(e):
                for f in self.q["pool"]:
                    f(e)

            @block.sync
            def _(e):
                for f in self.q["sp"]:
                    f(e)


def build_program(NCH, gC):
    nc = bass.Bass("TRN2", target_bir_lowering=False)

    def din(name, shape, dt=F32):
        return nc.dram_tensor(name, list(shape), dt, kind="ExternalInput").ap()

    T = NCH * 128
    x_d = din("x", [T, D])
    out_d = nc.dram_tensor("out", [T, D], F32, kind="ExternalOutput").ap()
    ccol_d = din("ccol", [128, 8])
    wada_d = din("w_ada", [D, 6 * D])
    bada_d = din("b_ada", [1, 6 * D])
    cols_d = din("cols", [128, 40])
    win_d = din("w_in", [D, 7168])
    wr_d = din("w_ret_out", [D, D])
    ws_d = din("w_sgu_out", [D, D])
    wo_d = din("w_out", [D, D])
    wq_d = din("w_q", [D, 2048])
    keys_d = din("keys", [16, 128, 128])
    sguw_d = din("sgu_w", [4, 128, 128])
    sgub_d = din("sgu_b", [1, 512])
    fg_d = din("final_g", [1, D])
    pu_d = din("peer_u", [16384, D])
    pv_d = din("peer_v", [16384, D])
    identb_d = din("identb", [128, 128], BF16)
    cmisc_d = din("cmisc", [128, 408])
    cs_d = din("cs", [NCH, 128, 256])
    winb_d = nc.dram_tensor("winb", [NPA, 128, KT * 512], BF16).ap()
    puv_d = nc.dram_tensor("puv", [16384, 2 * D], BF16).ap()

    with ExitStack() as es:
        S = Sched(nc, es)
        Wr = S.tile("Wr", [128, KT, D], BF16)
        Ws = S.tile("Ws", [128, KT, D], BF16)
        Wo = S.tile("Wo", [128, KT, D], BF16)
        wbuf = [S.tile("wbuf%d" % i, [128, KT, 512], BF16) for i in range(2)]
        keysT = S.tile("keysT", [128, 16, 128], BF16)
        wsT = S.tile("wsT", [128, 4, 128], BF16)
        bsBp = S.tile("bsBp", [128, 8, 128], F32)
        gate2b = S.tile("gate2b", [128, D], F32)
        fgb = S.tile("fgb", [128, D], F32)
        state = S.tile("state", [128, D], F32)
        stateb = S.tile("stateb", [128, D], BF16)
        identb = S.tile("identb_s", [128, 128], BF16)
        onesb = S.tile("onesb", [128, 128], BF16)
        cm = S.tile("cmisc_s", [128, 408], F32)
        cols = S.tile("cols_s", [128, 40], F32)
        modcol = S.tile("modcol", [128, 32], F32)
        acol = S.tile("acol", [128, 16], F32)
        xts = [S.tile("xt%d" % i, [128, D], F32) for i in range(3)]
        junkA = S.tile("junkA", [128, D], BF16)
        junkDs = [S.tile("junkD0", [128, D], BF16)] * 2
        sm = S.tile("smalls", [128, 64], F32)
        h2toks = [S.tile("h2tok%d" % i, [128, D], BF16) for i in range(2)]
        eidxs = [S.tile("eidx%d" % i, [128, 128], I32) for i in range(2)]
        ggs = [S.tile("gg%d" % i, [128, 8, 16], F32) for i in range(2)]
        pre = S.tile("pre", [128, 128], F32)
        glt = S.tile("glt", [128, 128], F32)
        glg = S.tile("glg", [128, 128], F32)
        dg = [S.tile("dg%d" % i, [128, 128], BF16) for i in range(4)]
        rtmp = S.tile("rtmp", [128, 512], F32)
        slots = [S.tile("slot%d" % i, [128, 2 * D], BF16) for i in range(NSLOT)]
        banks = [S.psum("bank%d" % i, [128, 512], F32) for i in range(8)]
        bank_rr = [0]

        def nb(n=1):
            r = []
            for _ in range(n):
                r.append(banks[bank_rr[0] % 4])
                bank_rr[0] += 1
            return r if n > 1 else r[0]

        trilT = cm.a[:, 0:128]
        dq = cm.a[:, 128:132]
        dk = cm.a[:, 132:136]
        iota16 = cm.a[:, 136:152]
        ones_f = cm.a[:, 152:280]
        identf = cm.a[:, 280:408]
        g1col, g2col = cols.a[:, 0:8], cols.a[:, 8:16]
        gncol, lngcol, lnbcol = cols.a[:, 16:24], cols.a[:, 24:32], cols.a[:, 32:40]
        sh1col, sc1col = modcol.a[:, 0:8], modcol.a[:, 8:16]
        sh2col, sc2col = modcol.a[:, 16:24], modcol.a[:, 24:32]
        a1col, a2col = acol.a[:, 0:8], acol.a[:, 8:16]

        ccol = S.carve("P1", "ccol", 32, F32)
        cact = S.carve("P1", "cact", 32, F32)
        wab = [S.carve("P1", "wab%d" % i, 4096, F32, "p (k n) -> p k n", k=KT) for i in range(2)]
        modrow = S.carve("P1", "modrow", 24576, F32)
        gate1b = S.carve("P1", "gate1b", 4096, F32)
        stage = [S.carve("P1", "stage0", 4096, F32)] * 2
        kst = S.carve("P2", "kst", 8192, F32, "p (k n) -> p k n", k=16)
        kstb = S.carve("P2", "kstb", 4096, BF16, "p (k n) -> p k n", k=16)
        wst = S.carve("P2", "wst", 2048, F32, "p (k n) -> p k n", k=4)
        wstb = S.carve("P2", "wstb", 1024, BF16, "p (k n) -> p k n", k=4)
        bsBs = S.carve("P2", "bsBs", 2048, F32, "p (k n) -> p k n", k=4)
        bsrow = S.carve("P2", "bsrow", 2048, F32)
        xn = S.carve("M", "xn", 2048, BF16)
        hT = S.carve("M", "hT", 2048, BF16, "p (k n) -> p k n", k=KT)
        cs = S.carve("M", "cs", 1024, F32)
        qk = S.carve("M", "qk", 4096, F32, "p (h n) -> p h n", h=8)
        rA = S.carve("M", "rA", 4096, F32, "p (h n) -> p h n", h=8)
        rB = S.carve("M", "rB", 4096, F32, "p (h n) -> p h n", h=8)
        qd = S.carve("M", "qd", 1024, BF16, "p (h n) -> p h n", h=4)
        kp = S.carve("M", "kp", 1024, BF16, "p (h n) -> p h n", h=4)
        qkT = S.carve("M", "qkT", 2048, BF16, "p (h n) -> p h n", h=8)
        vv = S.carve("M", "v", 2048, BF16)
        sg = S.carve("M", "sg", 4096, F32)
        guT = S.carve("M", "guT", 2048, BF16, "p (k n) -> p k n", k=KT)
        gsv = S.carve("M", "gsv", 4096, F32)
        vhat = S.carve("M", "vhat", 2048, BF16)
        sigA = S.carve("M", "sigA", 2048, BF16, "p (k n) -> p k n", k=KT)
        sigB = S.carve("M", "sigB", 2048, BF16, "p (k n) -> p k n", k=KT)
        sTm = S.carve("M", "sTm", 1024, BF16, "p (h n) -> p h n", h=4)
        zz = xn
        zT = hT
        ysT = qkT
        mergedT = guT
        xn2 = S.carve("E", "xn2", 2048, BF16)
        h2T = S.carve("E", "h2T", 2048, BF16, "p (k n) -> p k n", k=KT)
        qT = S.carve("E", "qT", 4096, BF16, "p (k n) -> p k n", k=16)
        W8 = S.carve("E", "W8", 8192, F32)
        v1 = S.carve("E", "v1", 1024, F32, "p (k n) -> p k n", k=16)
        i1 = S.carve("E", "i1", 1024, U32, "p (k n) -> p k n", k=16)
        i1f = S.carve("E", "i1f", 1024, F32, "p (k n) -> p k n", k=16)
        tsv = S.carve("E", "ts", 512, F32, "p (k n) -> p k n", k=8)
        tiv = S.carve("E", "ti", 512, U32, "p (k n) -> p k n", k=8)
        tab = S.carve("E", "tab", 1024, U32)
        abf = S.carve("E", "abf", 1024, F32)
        k12 = S.carve("E", "k12", 1024, F32)
        ef = S.carve("E", "ef", 512, F32)
        gex = S.carve("E", "gex", 512, F32, "p (k n) -> p k n", k=8)
        s3 = W8.a.rearrange("p (k n) -> p k n", k=16)
        s_hp = [S.sub(W8, s3[:, hp, :], "s%d" % hp) for hp in range(16)]
        c3 = W8.a.rearrange("p (k n) -> p k n", k=8)
        c_h = [S.sub(W8, c3[:, h, :], "c%d" % h) for h in range(8)]
        oh = S.sub(W8, W8.a.rearrange("p (h k j) -> p h k j", h=8, k=16), "oh")
        S.alias(s_hp, c_h)
        S.alias(s_hp, [oh])
        S.alias(c_h, [oh])
        v1_hp = [S.sub(v1, v1.a[:, hp, :]) for hp in range(16)]
        i1_hp = [S.sub(i1, i1.a[:, hp, :]) for hp in range(16)]
        ts_h = [S.sub(tsv, tsv.a[:, h, :]) for h in range(8)]
        ti_h = [S.sub(tiv, tiv.a[:, h, :]) for h in range(8)]
        pre_c = [S.sub(pre, pre.a[:, j:j + 1]) for j in range(128)]
        gl_c = [S.sub(glt, glt.a[:, j:j + 1]) for j in range(128)]
        glg_c = [S.sub(glg, glg.a[:, j:j + 1]) for j in range(128)]
        ss = Tk(sm.a[:, 0:1], "ss")
        rstd = Tk(sm.a[:, 1:2], "rstd")
        bst = Tk(sm.a[:, 2:26].rearrange("p (h s) -> p h s", h=4), "bst")
        bmv = Tk(sm.a[:, 26:34].rearrange("p (h s) -> p h s", h=4), "bmv")
        brs = Tk(sm.a[:, 34:38], "brs")
        lst = Tk(sm.a[:, 38:50], "lst")
        lmv = Tk(sm.a[:, 50:52], "lmv")
        lrs = Tk(sm.a[:, 52:53], "lrs")
        zsum = Tk(sm.a[:, 53:61], "zsum")
        ssb = Tk(sm.a[:, 61:62], "ssb")
        rstdb = Tk(sm.a[:, 62:63], "rstdb")

        def dma(eng, out, in_, key, reads=(), writes=()):
            S.op(eng, lambda e: e.dma_start(out=out, in_=in_), reads, writes, dma=key)

        def mm(out, lhsT, rhs, start, stop, reads, writes):
            S.op("pe", lambda e: e.matmul(out, lhsT=lhsT, rhs=rhs, start=start, stop=stop), reads, writes)

        def tr(out, in_, reads, writes):
            S.op("pe", lambda e: e.transpose(out=out, in_=in_, identity=identb.a), list(reads) + [identb], writes)

        def act(out, in_, func, reads, writes, **kw):
            S.op("act", lambda e: e.activation(out=out, in_=in_, func=func, **kw), reads, writes)

        def tt(eng, out, in0, in1, op, reads, writes):
            S.op(eng, lambda e: e.tensor_tensor(out=out, in0=in0, in1=in1, op=op), reads, writes)

        def tsc(eng, out, in0, s1, s2, op0, op1, reads, writes):
            if s2 is None:
                S.op(eng, lambda e: e.tensor_scalar(out=out, in0=in0, scalar1=s1, scalar2=None, op0=op0), reads, writes)
            else:
                S.op(eng, lambda e: e.tensor_scalar(out=out, in0=in0, scalar1=s1, scalar2=s2, op0=op0, op1=op1), reads, writes)

        def stt(eng, out, in0, scalar, in1, op0, op1, reads, writes, accum=None):
            if accum is None:
                S.op(eng, lambda e: e.scalar_tensor_tensor(out=out, in0=in0, scalar=scalar, in1=in1, op0=op0, op1=op1), reads, writes)
            else:
                S.op(eng, lambda e: e.scalar_tensor_tensor(out=out, in0=in0, scalar=scalar, in1=in1, op0=op0, op1=op1, accum_out=accum), reads, writes)

        def cp(eng, out, in_, reads, writes):
            if eng == "act":
                S.op(eng, lambda e: e.activation(out=out, in_=in_, func=AF.Copy), reads, writes)
            else:
                S.op(eng, lambda e: e.tensor_copy(out=out, in_=in_), reads, writes)

        def rstd_from(src_tk, src_ap, dst_tk, dst_ap, mul):
            tsc("dve", dst_ap, src_ap, mul, EPS, ALU.mult, ALU.add, [src_tk], [dst_tk])
            act(dst_ap, dst_ap, AF.Sqrt, [dst_tk], [dst_tk])
            S.op("dve", lambda e: e.reciprocal(out=dst_ap, in_=dst_ap), [dst_tk], [dst_tk])

        dma("sp", identb.a, identb_d, "c0", writes=[identb])
        dma("sp", cm.a, cmisc_d, "c1", writes=[cm])
        dma("sp", cols.a, cols_d, "c2", writes=[cols])
        dma("sp", ccol.a, ccol_d, "c3", writes=[ccol])
        dma("sp", modrow.a[0:1, :], bada_d, "c4", writes=[modrow])
        dma("sp", fgb.a, fg_d.partition_broadcast(128), "c5", writes=[fgb])
        S.op("dve", lambda e: e.memset(onesb.a, 1.0), writes=[onesb])
        S.op("dve", lambda e: e.memset(state.a, 0.0), writes=[state])
        S.op("dve", lambda e: e.memset(stateb.a, 0.0), writes=[stateb])
        winb_tk = [Tk(None, "winb%d" % j) for j in range(NPA)]
        win_v = win_d.rearrange("(kt p) n -> p kt n", p=128)
        wq_v = wq_d.rearrange("(kt p) n -> p kt n", p=128)
        for j in range(NPA):
            src = win_v[:, :, j * 512:(j + 1) * 512] if j < NPIECE else wq_v[:, :, (j - NPIECE) * 512:(j - NPIECE + 1) * 512]
            dma("pool", winb_d[j].rearrange("p (k n) -> p k n", k=KT), src, "wc", writes=[winb_tk[j]])
        for j in range(NPA):
            winb_tk[j].w = (S._dsem("wc"), 16 * NPA)
        ws_v = ws_d.rearrange("(kt p) n -> p kt n", p=128)
        for kt in range(KT):
            dma("pool", Ws.a[:, kt, :], ws_v[:, kt, :], "wsg", writes=[Ws])
        puv_tk = Tk(None, "puv")
        for r in range(16):
            dma("pool", puv_d[r * 1024:(r + 1) * 1024, 0:D], pu_d[r * 1024:(r + 1) * 1024, :], "uvc")
            dma("pool", puv_d[r * 1024:(r + 1) * 1024, D:2 * D], pv_d[r * 1024:(r + 1) * 1024, :], "uvc")
        puv_tk.w = (S._dsem("uvc"), 16 * 32)
        act(cact.a, ccol.a, AF.Silu, [ccol], [cact])
        wada_v = wada_d.rearrange("(kt p) n -> p kt n", p=128)
        for j in range(48):
            wb = wab[j % 2]
            dma("sp", wb.a, wada_v[:, :, j * 128:(j + 1) * 128], "wa%d" % (j % 2), writes=[wb])
            pb = nb()
            for kt in range(KT):
                mm(pb.a[0:1, 0:128], cact.a[:, kt:kt + 1], wb.a[:, kt, :], kt == 0, kt == KT - 1, [cact, wb], [pb])
            tt("dve", modrow.a[0:1, j * 128:(j + 1) * 128], pb.a[0:1, 0:128], modrow.a[0:1, j * 128:(j + 1) * 128],
               ALU.add, [pb, modrow], [modrow])
        pb = nb()
        for si, sec in enumerate((0, 1, 3, 4)):
            for kt in range(KT):
                c0 = sec * D + kt * 128
                mm(pb.a[:, si * 8 + kt:si * 8 + kt + 1], modrow.a[0:1, c0:c0 + 128], ones_f[0:1, 0:1], True, True,
                   [modrow, cm], [pb])
        cp("dve", modcol.a, pb.a[:, 0:32], [pb], [modcol])
        stt("dve", a1col, sc1col, 1.0, g1col, ALU.add, ALU.mult, [modcol, cols], [acol])
        stt("dve", a2col, sc2col, 1.0, g2col, ALU.add, ALU.mult, [modcol, cols], [acol])
        for half in range(2):
            pb = nb()
            mm(pb.a, ones_f[0:1, :], modrow.a[0:1, 2 * D + half * 512:2 * D + (half + 1) * 512], True, True, [modrow, cm], [pb])
            cp("act", gate1b.a[:, half * 512:(half + 1) * 512], pb.a, [pb], [gate1b])
            pb = nb()
            mm(pb.a, ones_f[0:1, :], modrow.a[0:1, 5 * D + half * 512:5 * D + (half + 1) * 512], True, True, [modrow, cm], [pb])
            cp("act", gate2b.a[:, half * 512:(half + 1) * 512], pb.a, [pb], [gate2b])
        wr_v = wr_d.rearrange("(kt p) n -> p kt n", p=128)
        wo_v = wo_d.rearrange("(kt p) n -> p kt n", p=128)
        for kt in range(KT):
            st_ = stage[0]
            dma("sp", st_.a, wr_v[:, kt, :], "st0", writes=[st_])
            act(Wr.a[:, kt, :], st_.a, AF.Copy, [st_, cols], [Wr], scale=gncol[:, kt:kt + 1])
            st_ = stage[1]
            dma("sp", st_.a, wo_v[:, kt, :], "st0", writes=[st_])
            tt("dve", Wo.a[:, kt, :], st_.a, gate1b.a, ALU.mult, [st_, gate1b], [Wo])
        dma("sp", kst.a, keys_d.rearrange("k n c -> n k c"), "ks", writes=[kst])
        cp("act", kstb.a, kst.a, [kst], [kstb])
        for g in range(2):
            pb = nb()
            pbb = pb.a.bitcast(BF16).rearrange("p (k n) -> p k n", k=8)
            for j in range(8):
                tr(pbb[:, j, :], kstb.a[:, g * 8 + j, :], [kstb], [pb])
            cp("dve", keysT.a[:, g * 8:(g + 1) * 8, :], pbb, [pb], [keysT])
        dma("sp", wst.a, sguw_d.rearrange("g t s -> t g s"), "wt", writes=[wst])
        dma("sp", bsrow.a[0:1, 0:512], sgub_d, "bs", writes=[bsrow])
        cp("act", wstb.a, wst.a, [wst], [wstb])
        pb = nb()
        pbb = pb.a.bitcast(BF16).rearrange("p (k n) -> p k n", k=8)
        for g in range(4):
            tr(pbb[:, g, :], wstb.a[:, g, :], [wstb], [pb])
        tt("dve", wsT.a, pbb[:, 0:4, :], trilT.unsqueeze(1).broadcast_to([128, 4, 128]), ALU.mult, [pb, cm], [wsT])
        pA = nb()
        pB = nb()
        pA3 = pA.a.rearrange("p (k n) -> p k n", k=4)
        pB3 = pB.a.rearrange("p (k n) -> p k n", k=4)
        for g in range(4):
            mm(pA3[:, g, :], onesb.a, wsT.a[:, g, :], True, True, [onesb, wsT], [pA])
            mm(pB3[:, g, :], ones_f[0:1, :], bsrow.a[0:1, g * 128:(g + 1) * 128], True, True, [cm, bsrow], [pB])
        cp("act", bsBs.a, pB3, [pB], [bsBs])
        for ct in range(8):
            stt("dve", bsBp.a[:, ct, :], pA3[:, ct // 2, :], lnbcol[:, ct:ct + 1], bsBs.a[:, ct // 2, :], ALU.mult, ALU.add,
                [pA, cols, bsBs], [bsBp])

        total_pieces = NCH * NPA
        issued = [0]

        def ensure_w(upto):
            while issued[0] <= min(upto, total_pieces - 1):
                g_ = issued[0]
                j_ = g_ % NPA
                wb_ = wbuf[g_ % 2]
                dma("sp", wb_.a, winb_d[j_].rearrange("p (k n) -> p k n", k=KT), "wb%d" % (g_ % 2),
                    reads=[winb_tk[j_]], writes=[wb_])
                issued[0] += 1

        dma("sp", xts[0].a, x_d[0:128, :], "x0", writes=[xts[0]])
        dma("sp", cs.a, cs_d[0], "cs", writes=[cs])
        ensure_w(0)

        def norm_transpose(xt, xn_t, hT_t, acolv, shcolv):
            S.op("dve", lambda e: e.memset(ss.a, 0.0), writes=[ss])
            act(junkA.a, xt.a, AF.Square, [xt, ss], [ss, junkA], accum_out=ss.a)
            rstd_from(ss, ss.a, rstd, rstd.a, 1.0 / D)
            act(xn_t.a, xt.a, AF.Copy, [xt, rstd], [xn_t], scale=rstd.a)
            pb = nb()
            pbb = pb.a.bitcast(BF16).rearrange("p (k n) -> p k n", k=8)
            for kt in range(KT):
                tr(pbb[:, kt, :], xn_t.a[:, kt * 128:(kt + 1) * 128], [xn_t], [pb])
            for kt in range(KT):
                tsc("dve", hT_t.a[:, kt, :], pbb[:, kt, :], acolv[:, kt:kt + 1], shcolv[:, kt:kt + 1], ALU.mult, ALU.add,
                    [pb, acol, modcol], [hT_t])

        def front(i):
            xt = xts[i % 3]
            h2tok, eidx, gg = h2toks[i % 2], eidxs[i % 2], ggs[i % 2]
            norm_transpose(xt, xn, hT, a1col, sh1col)
            yield
            for j in range(NPIECE):
                gidx = i * NPA + j
                ensure_w(gidx + 1)
                wb = wbuf[gidx % 2]
                pb = nb()
                fm = j in (6, 7, 10, 11, 12, 13)
                if not fm:
                    for kt in range(KT):
                        mm(pb.a, hT.a[:, kt, :], wb.a[:, kt, :], kt == 0, kt == KT - 1, [hT, wb], [pb])
                else:
                    p3 = pb.a.rearrange("p (k n) -> p k n", k=4)
                    for n in range(4):
                        for kt in range(KT):
                            mm(p3[:, n, :], wb.a[:, kt, n * 128:(n + 1) * 128], hT.a[:, kt, :], kt == 0, kt == KT - 1,
                               [hT, wb], [pb])
                if j < 2:
                    act(qk.a[:, j * 4:(j + 1) * 4, :], pb.a.rearrange("p (h n) -> p h n", h=4), AF.Copy, [pb], [qk])
                elif j < 4:
                    act(vv.a[:, (j - 2) * 512:(j - 1) * 512], pb.a, AF.Copy, [pb], [vv])
                elif j < 6:
                    act(sg.a[:, (j - 4) * 512:(j - 3) * 512], pb.a, AF.Silu, [pb], [sg])
                elif j < 8:
                    act(guT.a[:, (j - 6) * 4:(j - 5) * 4, :], p3, AF.Gelu, [pb], [guT])
                elif j < 10:
                    act(gsv.a[:, (j - 8) * 512:(j - 7) * 512], pb.a, AF.Gelu, [pb], [gsv])
                elif j < 12:
                    act(sigA.a[:, (j - 10) * 4:(j - 9) * 4, :], p3, AF.Sigmoid, [pb], [sigA])
                else:
                    act(sigB.a[:, (j - 12) * 4:(j - 11) * 4, :], p3, AF.Sigmoid, [pb], [sigB])
                if j == 1:
                    c2 = cs.a[:, 0:128].unsqueeze(1).broadcast_to([128, 8, 128])
                    sn = cs.a[:, 128:192].unsqueeze(1).broadcast_to([128, 8, 64])
                    sp_ = cs.a[:, 192:256].unsqueeze(1).broadcast_to([128, 8, 64])
                    tt("dve", rA.a, qk.a, c2, ALU.mult, [qk, cs], [rA])
                    tt("dve", rB.a[:, :, 0:64], qk.a[:, :, 64:128], sn, ALU.mult, [qk, cs], [rB])
                    tt("dve", rB.a[:, :, 64:128], qk.a[:, :, 0:64], sp_, ALU.mult, [qk, cs], [rB])
                    tt("dve", rA.a, rA.a, rB.a, ALU.add, [rA, rB], [rA])
                    yield
                    for h in range(4):
                        act(qd.a[:, h, :], rA.a[:, h, :], AF.Copy, [rA, cm], [qd], scale=dq[:, h:h + 1])
                        act(kp.a[:, h, :], rA.a[:, 4 + h, :], AF.Copy, [rA, cm], [kp], scale=dk[:, h:h + 1])
                    pt_ = nb()
                    ptb = pt_.a.bitcast(BF16).rearrange("p (k n) -> p k n", k=8)
                    for h in range(4):
                        tr(ptb[:, h, :], qd.a[:, h, :], [qd], [pt_])
                        tr(ptb[:, 4 + h, :], kp.a[:, h, :], [kp], [pt_])
                    cp("act", qkT.a, ptb, [pt_], [qkT])
                    psc = nb()
                    ps3 = psc.a.rearrange("p (h n) -> p h n", h=4)
                    for h in range(4):
                        mm(ps3[:, h, :], qkT.a[:, 4 + h, :], qkT.a[:, h, :], True, True, [qkT], [psc])
                    tt("dve", sTm.a, ps3, trilT.unsqueeze(1).broadcast_to([128, 4, 128]), ALU.mult, [psc, cm], [sTm])
                yield
            pY = nb(2)
            pD = nb(2)
            for h in range(4):
                o = pY[h // 2].a[:, (h % 2) * 256:(h % 2 + 1) * 256]
                mm(o, sTm.a[:, h, :], vv.a[:, h * 256:(h + 1) * 256], True, False, [sTm, vv], [pY[h // 2]])
                mm(o, qkT.a[:, h, :], stateb.a[:, h * 256:(h + 1) * 256], False, True, [qkT, stateb], [pY[h // 2]])
            for h in range(4):
                o = pD[h // 2].a[:, (h % 2) * 256:(h % 2 + 1) * 256]
                mm(o, kp.a[:, h, :], vv.a[:, h * 256:(h + 1) * 256], True, True, [kp, vv], [pD[h // 2]])
            tmpS_a = qk.a.rearrange("p h n -> p (h n)")
            for hh in range(2):
                tt("dve", tmpS_a[:, hh * 512:(hh + 1) * 512], pD[hh].a, state.a[:, hh * 512:(hh + 1) * 512], ALU.add,
                   [pD[hh], state], [qk])
            for h in range(4):
                act(state.a[:, h * 256:(h + 1) * 256], tmpS_a[:, h * 256:(h + 1) * 256], AF.Copy, [qk], [state],
                    scale=float(gC[h]))
            cp("act", stateb.a, state.a, [state], [stateb])
            for h in range(4):
                src = pY[h // 2].a[:, (h % 2) * 256:(h % 2 + 1) * 256]
                S.op("dve", lambda e, src=src, h=h: e.bn_stats(out=bst.a[:, h, :], in_=src), [pY[h // 2]], [bst])
            for h in range(4):
                S.op("dve", lambda e, h=h: e.bn_aggr(out=bmv.a[:, h, :], in_=bst.a[:, h, :]), [bst], [bmv])
            rstd_from(bmv, bmv.a[:, :, 1], brs, brs.a, 1.0)
            yn = rA.a.rearrange("p h n -> p (h n)")
            for h in range(4):
                src = pY[h // 2].a[:, (h % 2) * 256:(h % 2 + 1) * 256]
                tsc("dve", yn[:, h * 256:(h + 1) * 256], src, bmv.a[:, h, 0:1], brs.a[:, h:h + 1], ALU.subtract, ALU.mult,
                    [pY[h // 2], bmv, brs], [rA])
            tt("dve", zz.a, sg.a, yn, ALU.mult, [sg, rA], [zz])
            yield
            pt_ = nb()
            ptb = pt_.a.bitcast(BF16).rearrange("p (k n) -> p k n", k=8)
            for kt in range(KT):
                tr(ptb[:, kt, :], zz.a[:, kt * 128:(kt + 1) * 128], [zz], [pt_])
            cp("act", zT.a, ptb, [pt_], [zT])
            pA_ = nb(2)
            for n in range(8):
                o = pA_[n // 4].a.rearrange("p (k n) -> p k n", k=4)[:, n % 4, :]
                for kt in range(KT):
                    mm(o, Wr.a[:, kt, n * 128:(n + 1) * 128], zT.a[:, kt, :], kt == 0, kt == KT - 1, [Wr, zT], [pA_[n // 4]])
                if n == 3:
                    yield
            m1 = gsv.a.rearrange("p (k n) -> p k n", k=KT)
            m2 = sg.a.rearrange("p (k n) -> p k n", k=KT)
            for c in range(2):
                S.op("dve", lambda e, c=c: e.bn_stats(out=lst.a[:, c * 6:(c + 1) * 6], in_=gsv.a[:, c * 512:(c + 1) * 512]),
                     [gsv], [lst])
            S.op("dve", lambda e: e.bn_aggr(out=lmv.a, in_=lst.a), [lst], [lmv])
            rstd_from(lmv, lmv.a[:, 1:2], lrs, lrs.a, 1.0)
            tsc("dve", vhat.a, gsv.a, lmv.a[:, 0:1], lrs.a, ALU.subtract, ALU.mult, [gsv, lmv, lrs], [vhat])
            yield
            for hh in range(2):
                tt("dve", m1[:, hh * 4:(hh + 1) * 4, :], pA_[hh].a.rearrange("p (k n) -> p k n", k=4),
                   sigA.a[:, hh * 4:(hh + 1) * 4, :], ALU.mult, [pA_[hh], sigA], [gsv])
            pM = nb(2)
            for ct in range(8):
                o = pM[ct // 4].a.rearrange("p (k n) -> p k n", k=4)[:, ct % 4, :]
                mm(o, vhat.a[:, ct * 128:(ct + 1) * 128], wsT.a[:, ct // 2, :], True, True, [vhat, wsT], [pM[ct // 4]])
            t1 = rB.a
            for ct in range(8):
                src = pM[ct // 4].a.rearrange("p (k n) -> p k n", k=4)[:, ct % 4, :]
                stt("dve", t1[:, ct, :], src, lngcol[:, ct:ct + 1], bsBp.a[:, ct, :], ALU.mult, ALU.add,
                    [pM[ct // 4], cols, bsBp], [rB])
            tt("dve", ysT.a, guT.a, t1, ALU.mult, [guT, rB], [ysT])
            yield
            pB_ = nb(2)
            for n in range(8):
                o = pB_[n // 4].a.rearrange("p (k n) -> p k n", k=4)[:, n % 4, :]
                for kt in range(KT):
                    mm(o, Ws.a[:, kt, n * 128:(n + 1) * 128], ysT.a[:, kt, :], kt == 0, kt == KT - 1, [Ws, ysT], [pB_[n // 4]])
                if n == 3:
                    yield
            for hh in range(2):
                tt("dve", m2[:, hh * 4:(hh + 1) * 4, :], pB_[hh].a.rearrange("p (k n) -> p k n", k=4),
                   sigB.a[:, hh * 4:(hh + 1) * 4, :], ALU.mult, [pB_[hh], sigB], [sg])
            tt("dve", mergedT.a, m1, m2, ALU.add, [gsv, sg], [mergedT])
            yield
            pO = nb(2)
            for hh in range(2):
                for kt in range(KT):
                    mm(pO[hh].a, mergedT.a[:, kt, :], Wo.a[:, kt, hh * 512:(hh + 1) * 512], kt == 0, kt == KT - 1,
                       [mergedT, Wo], [pO[hh]])
            for hh in range(2):
                tt("dve", xt.a[:, hh * 512:(hh + 1) * 512], pO[hh].a, xt.a[:, hh * 512:(hh + 1) * 512], ALU.add,
                   [pO[hh], xt], [xt])
            yield
            norm_transpose(xt, xn2, h2T, a2col, sh2col)
            pt_ = nb()
            ptb = pt_.a.bitcast(BF16)
            for kt in range(KT):
                tr(ptb[:, kt * 128:(kt + 1) * 128], h2T.a[:, kt, :], [h2T], [pt_])
            cp("act", h2tok.a, ptb, [pt_], [h2tok])
            yield
            for p_ in range(4):
                gidx = i * NPA + NPIECE + p_
                ensure_w(gidx + 1)
                wb = wbuf[gidx % 2]
                pb = nb()
                p3 = pb.a.rearrange("p (k n) -> p k n", k=4)
                for n in range(4):
                    for kt in range(KT):
                        mm(p3[:, n, :], wb.a[:, kt, n * 128:(n + 1) * 128], h2T.a[:, kt, :], kt == 0, kt == KT - 1,
                           [h2T, wb], [pb])
                cp("act", qT.a[:, p_ * 4:(p_ + 1) * 4, :], p3, [pb], [qT])
                yield
            for g in range(4):
                pb = nb()
                p3 = pb.a.rearrange("p (k n) -> p k n", k=4)
                for n in range(4):
                    hp = g * 4 + n
                    mm(p3[:, n, :], qT.a[:, hp, :], keysT.a[:, hp, :], True, True, [qT, keysT], [pb])
                cp("act", s3[:, g * 4:(g + 1) * 4, :], p3, [pb], s_hp[g * 4:(g + 1) * 4])
            yield
            for rnd in range(2):
                lo, hi = rnd * 8, rnd * 8 + 8
                for hp in range(16):
                    S.op("dve", lambda e, hp=hp, lo=lo, hi=hi: e.max(out=v1.a[:, hp, lo:hi], in_=s3[:, hp, :]),
                         [s_hp[hp]], [v1_hp[hp]])
                    if hp % 8 == 7:
                        yield
                for hp in range(16):
                    S.op("dve", lambda e, hp=hp, lo=lo, hi=hi: e.max_index(out=i1.a[:, hp, lo:hi], in_max=v1.a[:, hp, lo:hi],
                                                                          in_values=s3[:, hp, :]),
                         [s_hp[hp], v1_hp[hp]], [i1_hp[hp]])
                    if hp % 8 == 7:
                        yield
                if rnd == 0:
                    for hp in range(16):
                        S.op("dve", lambda e, hp=hp: e.match_replace(out=s3[:, hp, :], in_to_replace=v1.a[:, hp, 0:8],
                                                                     in_values=s3[:, hp, :], imm_value=-1e30),
                             [v1_hp[hp]], [s_hp[hp]])
                        if hp % 8 == 7:
                            yield
            v14 = v1.a.rearrange("p (h two) k -> p h two k", two=2)
            c4 = W8.a.rearrange("p (h a b) -> p h a b", h=8, a=16)
            tt("dve", c4, v14[:, :, 0, :].unsqueeze(3).broadcast_to([128, 8, 16, 16]),
               v14[:, :, 1, :].unsqueeze(2).broadcast_to([128, 8, 16, 16]), ALU.add, v1_hp, c_h)
            yield
            for rnd in range(2):
                lo, hi = rnd * 8, rnd * 8 + 8
                for h in range(8):
                    S.op("dve", lambda e, h=h, lo=lo, hi=hi: e.max(out=tsv.a[:, h, lo:hi], in_=c3[:, h, :]), [c_h[h]], [ts_h[h]])
                for h in range(8):
                    S.op("dve", lambda e, h=h, lo=lo, hi=hi: e.max_index(out=tiv.a[:, h, lo:hi], in_max=tsv.a[:, h, lo:hi],
                                                                        in_values=c3[:, h, :]),
                         [c_h[h], ts_h[h]], [ti_h[h]])
                yield
                if rnd == 0:
                    for h in range(8):
                        S.op("dve", lambda e, h=h: e.match_replace(out=c3[:, h, :], in_to_replace=tsv.a[:, h, 0:8],
                                                                   in_values=c3[:, h, :], imm_value=-1e30),
                             [ts_h[h]], [c_h[h]])
                yield
            ti2 = tiv.a.rearrange("p h k -> p (h k)")
            S.op("dve", lambda e: e.tensor_single_scalar(out=tab.a[:, 0:128], in_=ti2, scalar=4, op=ALU.logical_shift_right),
                 ti_h, [tab])
            S.op("dve", lambda e: e.tensor_single_scalar(out=tab.a[:, 128:256], in_=ti2, scalar=15, op=ALU.bitwise_and),
                 ti_h, [tab])
            cp("dve", abf.a, tab.a, [tab], [abf])
            cp("dve", i1f.a, i1.a, i1_hp, [i1f])
            i1f4 = i1f.a.rearrange("p (h two) k -> p h two k", two=2)
            io4 = iota16.unsqueeze(1).unsqueeze(1).broadcast_to([128, 8, 16, 16])
            for p_ in range(2):
                sel = abf.a[:, p_ * 128:(p_ + 1) * 128].rearrange("p (h k) -> p h k", h=8)
                tt("dve", oh.a, sel.unsqueeze(3).broadcast_to([128, 8, 16, 16]), io4, ALU.is_equal, [abf, cm], [oh])
                yield
                tt("dve", oh.a, oh.a, i1f4[:, :, p_, :].unsqueeze(2).broadcast_to([128, 8, 16, 16]), ALU.mult, [oh, i1f], [oh])
                yield
                S.op("dve", lambda e, p_=p_: e.tensor_reduce(out=k12.a[:, p_ * 128:(p_ + 1) * 128],
                                                            in_=oh.a.rearrange("p h k j -> p (h k) j"), axis=AX.X, op=ALU.add),
                     [oh], [k12])
                yield
            stt("dve", ef.a, k12.a[:, 0:128], 128.0, k12.a[:, 128:256], ALU.mult, ALU.add, [k12], [ef])
            tsc("dve", ef.a, ef.a, 0.0, 16383.0, ALU.max, ALU.min, [ef], [ef])
            cp("dve", eidx.a, ef.a, [ef], [eidx])
            tt("dve", gex.a, tsv.a, tsv.a[:, :, 0:1].broadcast_to([128, 8, 16]), ALU.subtract, ts_h, [gex])
            act(gex.a, gex.a, AF.Exp, [gex], [gex])
            S.op("dve", lambda e: e.tensor_reduce(out=zsum.a, in_=gex.a, axis=AX.X, op=ALU.add), [gex], [zsum])
            S.op("dve", lambda e: e.reciprocal(out=zsum.a, in_=zsum.a), [zsum], [zsum])
            tt("dve", gg.a, gex.a, zsum.a.unsqueeze(2).broadcast_to([128, 8, 16]), ALU.mult, [gex, zsum], [gg])
            yield

        gi = [0]
        def back(i):
            pP = banks[4:6] if i % 2 == 0 else banks[6:8]
            xt = xts[i % 3]
            h2tok, eidx, gg = h2toks[i % 2], eidxs[i % 2], ggs[i % 2]
            ggf = gg.a.rearrange("p h k -> p (h k)")
            S.op("dve", lambda e: e.memset(pre.a, 0.0), writes=pre_c)
            G = 2
            NG = 128 // G
            hk_slot = {}

            def stageA(g):
                hks = list(range(g * G, (g + 1) * G))
                for hk in reversed(hks):
                    si = (i * 128 + hk) % NSLOT
                    sl = slots[si]
                    hk_slot[hk] = sl
                    S.op("pool", lambda e, sl=sl, hk=hk: e.indirect_dma_start(
                        out=sl.a, out_offset=None, in_=puv_d,
                        in_offset=bass.IndirectOffsetOnAxis(ap=eidx.a[:, hk:hk + 1], axis=0)),
                        [eidx, puv_tk], [sl], dma="g%d" % si)
                for hk in hks:
                    sl = hk_slot[hk]
                    jd = junkDs[hk % 2]
                    stt("dve", jd.a, sl.a[:, 0:D], 1.0, h2tok.a, ALU.mult, ALU.mult, [sl, h2tok, pre_c[hk]],
                        [pre_c[hk], jd], accum=pre.a[:, hk:hk + 1])

            def stageC(g):
                c0, c1 = g * G, (g + 1) * G
                act(glt.a[:, c0:c1], pre.a[:, c0:c1], AF.Gelu, pre_c[c0:c1], gl_c[c0:c1])
                for hk in range(c0, c1):
                    act(glg.a[:, hk:hk + 1], glt.a[:, hk:hk + 1], AF.Copy, [gl_c[hk], gg], [glg_c[hk]], scale=ggf[:, hk:hk + 1])
                for hk in range(c0, c1):
                    sl = hk_slot.pop(hk)
                    dgt = dg[hk % 4]
                    act(dgt.a, identf, AF.Copy, [cm, glg_c[hk]], [dgt], scale=glg.a[:, hk:hk + 1])
                    for hh in range(2):
                        mm(pP[hh].a, dgt.a, sl.a[:, D + hh * 512:D + (hh + 1) * 512], hk == 0, hk == 127, [dgt, sl], [pP[hh]])
                    sl.r.pop(S.sem["dve"], None)

            for step in range(NG + 1):
                if step < NG:
                    stageA(step)
                if step >= 1:
                    stageC(step - 1)
                yield
            yield "T"
            for hh in range(2):
                tt("dve", rtmp.a, pP[hh].a, gate2b.a[:, hh * 512:(hh + 1) * 512], ALU.mult, [pP[hh], gate2b], [rtmp])
                tt("dve", xt.a[:, hh * 512:(hh + 1) * 512], rtmp.a, xt.a[:, hh * 512:(hh + 1) * 512], ALU.add, [rtmp, xt], [xt])
            S.op("dve", lambda e: e.memset(ssb.a, 0.0), writes=[ssb])
            act(junkA.a, xt.a, AF.Square, [xt, ssb], [ssb, junkA], accum_out=ssb.a)
            rstd_from(ssb, ssb.a, rstdb, rstdb.a, 1.0 / D)
            stt("dve", xt.a, xt.a, rstdb.a, fgb.a, ALU.mult, ALU.mult, [xt, rstdb, fgb], [xt])
            S.op("act", lambda e, i=i, xt=xt: e.dma_start(out=out_d[i * 128:(i + 1) * 128, :], in_=xt.a), [xt], [], dma="out%d" % (i % 3))
            yield

        def prefetch_x(i):
            if i < NCH:
                dma("sp", xts[i % 3].a, x_d[i * 128:(i + 1) * 128, :], "x%d" % (i % 3), writes=[xts[i % 3]])

        def drain(g):
            for _ in g:
                pass

        drain(front(0))
        FRAC = 0.9
        RUNMAX = 4
        DVEW = 2.5
        KDEF = 4
        nbk = 128 // 2 + 1
        pending = None
        for i in range(NCH):
            b = back(i)
            bd = 0
            for _ in range(KDEF):
                next(b)
                bd += 1
            if pending is not None:
                drain(pending)
            bdone = False

            def step_back():
                nonlocal_state = None
                return next(b)

            if i + 1 < NCH:
                prefetch_x(i + 1)
                dma("sp", cs.a, cs_d[i + 1], "cs", writes=[cs])
                S.rec = []
                drain(front(i + 1))
                ops, S.rec = S.rec, None
                n_ops = len(ops)
                wts = [DVEW if o[0] == "dve" else 1.0 for o in ops]
                wtot = sum(wts)
                wacc = 0.0
                run = 0
                for k, (eng, fn, rd, wr, dm) in enumerate(ops):
                    S.op(eng, fn, rd, wr, dm)
                    run += 1
                    wacc += wts[k]
                    if k + 1 < n_ops and ops[k + 1][0] == eng and run < RUNMAX:
                        continue
                    run = 0
                    while not bdone and bd < KDEF + (wacc / wtot) * FRAC * (nbk - KDEF):
                        if next(b) == "T":
                            bdone = True
                        else:
                            bd += 1
            while not bdone:
                if next(b) == "T":
                    bdone = True
            pending = b
        drain(pending)

        fin = []
        for kk in ("out0", "out1", "out2"):
            if kk in S.dsem:
                fin.append((S.dsem[kk], S.cnt[S.dsem[kk]]))
        S.finish(fin)
        S.run()
    return nc, S.nins


def _consts(NCH):
    H, C = 4, 128
    gamma = 1.0 - 2.0 ** (-5.0 - np.arange(H, dtype=np.float64))
    i = np.arange(C, dtype=np.float64)
    cm = np.zeros((128, 408), np.float32)
    cm[:, 0:128] = (np.arange(128)[None, :] >= np.arange(128)[:, None]).astype(np.float32)
    cm[:, 128:132] = (gamma[None, :] ** (i[:, None] + 1.0)).astype(np.float32)
    cm[:, 132:136] = (gamma[None, :] ** (-(i[:, None] + 1.0)) * (128.0 ** -0.5)).astype(np.float32)
    cm[:, 136:152] = np.arange(16, dtype=np.float32)[None, :]
    cm[:, 152:280] = 1.0
    cm[:, 280:408] = np.eye(128, dtype=np.float32)
    gC = (gamma ** C).astype(np.float64)
    half = 64
    inv = (np.float32(10000.0) ** (-np.arange(half, dtype=np.float32) * np.float32(2.0) / np.float32(128))).astype(np.float32)
    pos = np.arange(NCH * 128, dtype=np.float32)
    ang = (pos[:, None] * inv[None, :]).astype(np.float32)
    cos, sin = np.cos(ang).astype(np.float32), np.sin(ang).astype(np.float32)
    cs = np.concatenate([cos, cos, -sin, sin], axis=1).reshape(NCH, 128, 256).astype(np.float32)
    identb = np.eye(128, dtype=np.float32).astype(ml_dtypes.bfloat16)
    return cm, gC, cs, identb


def _col(v):
    return np.ascontiguousarray(np.asarray(v, np.float32).reshape(8, 128).T)


_CACHE = {}


def run_cores(inputs, NCH, cores):
    f = lambda k: np.asarray(inputs[k], np.float32)
    cm, gC, cs, identb = _consts(NCH)
    key = NCH
    if key not in _CACHE:
        _CACHE[key] = build_program(NCH, gC)
    nc, nins = _CACHE[key]
    cols = np.concatenate([_col(f("norm1_g")[0]), _col(f("norm2_g")[0]), _col(f("ret_gn_g")[0]),
                           _col(f("sgu_ln_g")[0]), _col(f("sgu_ln_b")[0])], axis=1)
    shared = {
        "w_ada": np.ascontiguousarray(f("w_ada")[0]), "b_ada": np.ascontiguousarray(f("b_ada")[0].reshape(1, -1)),
        "cols": np.ascontiguousarray(cols), "w_in": np.ascontiguousarray(f("w_in")[0]),
        "w_ret_out": np.ascontiguousarray(f("w_ret_out")[0]), "w_sgu_out": np.ascontiguousarray(f("w_sgu_out")[0]),
        "w_out": np.ascontiguousarray(f("w_out")[0]), "w_q": np.ascontiguousarray(f("peer_w_q")[0]),
        "keys": np.ascontiguousarray(f("peer_sub_keys")[0].reshape(16, 128, 128)),
        "sgu_w": np.ascontiguousarray(f("sgu_w")[0]), "sgu_b": np.ascontiguousarray(f("sgu_b")[0].reshape(1, 512)),
        "final_g": np.ascontiguousarray(f("final_g").reshape(1, -1)),
        "peer_u": np.ascontiguousarray(f("peer_u")[0]), "peer_v": np.ascontiguousarray(f("peer_v")[0]),
        "identb": identb, "cmisc": cm, "cs": cs,
    }
    x, c = f("x"), f("c")
    in_maps = []
    for b in cores:
        m = dict(shared)
        m["x"] = np.ascontiguousarray(x[b, :NCH * 128])
        m["ccol"] = _col(c[b])
        in_maps.append(m)
    res = run_bass_kernel_spmd(nc, in_maps, core_ids=list(range(len(cores))))
    return np.stack([np.asarray(r["out"], np.float32) for r in res.results], axis=0)


def kernel(**inputs):
    return run_cores(inputs, 32, list(range(8))).astype(np.float32)
```
